# Optimizing a Trainium2 kernel written in Bass

```python
import jax, jax.numpy as jnp
from jax import lax
import numpy as np

D_MODEL = 1024
BATCH = 4
SEQ = 4096
DEPTH = 2

GRID_W = 64
CTX_LEN = 256
N_ADA = 6
HG_HEADS = 8
HG_DK = 128
HG_DV = 128
HG_CHUNK = 64
CONV_CH = 1024
CONV_K = 31
ATT_HEADS = 8
ATT_KV_HEADS = 4
ATT_GROUP = ATT_HEADS // ATT_KV_HEADS
HEAD_DIM = 128
ROPE_THETA = 10000.0
Q_BLOCK = 128
PEER_HEADS = 8
PEER_NKEYS = 128
PEER_EXPERTS = PEER_NKEYS * PEER_NKEYS
PEER_DQ = 256
PEER_TOPK = 16
PEER_BLOCK = 128

N_BRANCH = 3
EPS = 1e-6
ALPHA = (2 * DEPTH) ** 0.25
BETA = (8 * DEPTH) ** -0.25

IN_SPLITS = (
    ('hg_q', HG_HEADS * HG_DK),
    ('hg_f_fwd', HG_HEADS * HG_DK),
    ('hg_f_bwd', HG_HEADS * HG_DK),
    ('hg_i', HG_HEADS * HG_DV),
    ('hg_g', HG_HEADS * HG_DV),
    ('conv_glu', 2 * CONV_CH),
    ('att_q', ATT_HEADS * HEAD_DIM),
    ('att_k', ATT_KV_HEADS * HEAD_DIM),
    ('att_v', ATT_KV_HEADS * HEAD_DIM),
    ('merge_gate', N_BRANCH * D_MODEL),
)
IN_WIDTH = sum(w for _, w in IN_SPLITS)

kernel_name = 'hybrid_hgrn2_conformer_gqa_peer_dit'


def _split_in(z):
    out, off = {}, 0
    for name, w in IN_SPLITS:
        out[name] = z[..., off:off + w]
        off += w
    return out


def _layer_norm(x, g, b):
    xf = x.astype(jnp.float32)
    mu = jnp.mean(xf, -1, keepdims=True)
    var = jnp.mean(jnp.square(xf - mu), -1, keepdims=True)
    return ((xf - mu) * lax.rsqrt(var + EPS)).astype(x.dtype) * g + b


def _rms_norm(x, g):
    xf = x.astype(jnp.float32)
    return (xf * lax.rsqrt(jnp.mean(jnp.square(xf), -1, keepdims=True) + EPS)).astype(x.dtype) * g


def _modulate(x, shift, scale):
    return x * (1.0 + scale) + shift


def _hgrn_gates(z, lb):
    zf = z.astype(jnp.float32)
    f = lb + (1.0 - lb) * jax.nn.sigmoid(zf)
    log_f = jnp.log(jnp.maximum(f, jnp.finfo(jnp.float32).tiny))
    k = (1.0 - lb) * jax.nn.sigmoid(-zf)
    return log_f, k.astype(z.dtype)


def _hgrn_prep(parts, lb_fwd, lb_bwd):
    B, L = parts['hg_q'].shape[:2]
    heads = lambda a: a.reshape(B, L, HG_HEADS, -1)
    q = heads(jax.nn.silu(parts['hg_q']))
    v = heads(parts['hg_i'])
    lf_f, k_f = _hgrn_gates(parts['hg_f_fwd'], lb_fwd)
    lf_b, k_b = _hgrn_gates(parts['hg_f_bwd'], lb_bwd)
    return q, v, heads(lf_f), heads(k_f), heads(lf_b), heads(k_b)


def _hgrn_chunk_scan(q, k, v, log_f, s0):
    out_dtype = v.dtype
    B, L, H, _ = q.shape
    n = L // HG_CHUNK
    def chunks(a):
        a = a.astype(jnp.float32)
        return a.reshape(B, n, HG_CHUNK, H, a.shape[-1]).transpose(1, 0, 3, 2, 4)
    qc, kc, vc, fc = chunks(q), chunks(k), chunks(v), chunks(log_f)
    causal = jnp.tril(jnp.ones((HG_CHUNK, HG_CHUNK), bool))[:, :, None]
    def step(S, inp):
        qb, kb, vb, lf = inp
        b = jnp.cumsum(lf, axis=-2)
        diff = b[..., :, None, :] - b[..., None, :, :]
        decay = jnp.where(causal, jnp.exp(jnp.where(causal, diff, 0.0)), 0.0)
        scores = jnp.einsum('bhtd,bhtsd,bhsd->bhts', qb, decay, kb)
        o = jnp.einsum('bhts,bhsv->bhtv', scores, vb) + jnp.einsum('bhtd,bhdv->bhtv', qb * jnp.exp(b), S)
        b_last = b[..., -1, :]
        S_new = jnp.exp(b_last)[..., None] * S + jnp.einsum(
            'bhsd,bhsv->bhdv', kb * jnp.exp(b_last[..., None, :] - b), vb)
        return S_new, o
    s_fin, o = lax.scan(step, s0, (qc, kc, vc, fc))
    o = o.transpose(1, 0, 3, 2, 4).reshape(B, L, H, v.shape[-1])
    return o.astype(out_dtype), s_fin


def _hgrn_bidir(q, v, lf_f, k_f, lf_b, k_b, s_f, s_b):
    o_f, s_f = _hgrn_chunk_scan(q, k_f, v, lf_f, s_f)
    rev = lambda a: jnp.flip(a, axis=1)
    o_b, s_b = _hgrn_chunk_scan(rev(q), rev(k_b), rev(v), rev(lf_b), s_b)
    return o_f + rev(o_b), s_f, s_b


def _hgrn_out(o, gate, norm_g, w_o):
    B, L = o.shape[:2]
    o = _rms_norm(o, norm_g).reshape(B, L, HG_HEADS * HG_DV)
    return (o * jax.nn.silu(gate)) @ w_o


def _conformer_conv(glu_in, dw, db, ln_g, ln_b, w_o):
    a, g = jnp.split(glu_in, 2, axis=-1)
    u = a * jax.nn.sigmoid(g)
    u = lax.conv_general_dilated(
        u, dw[:, None, :], window_strides=(1,), padding=[(CONV_K // 2, CONV_K // 2)],
        dimension_numbers=('NWC', 'WIO', 'NWC'), feature_group_count=CONV_CH) + db
    u = jax.nn.silu(_layer_norm(u, ln_g, ln_b))
    return u @ w_o


def _grid_angles(L):
    n_rows = L // GRID_W
    row = jnp.repeat(jnp.arange(n_rows), GRID_W).astype(jnp.float32)
    col = jnp.tile(jnp.arange(GRID_W), n_rows).astype(jnp.float32)
    n_freq = HEAD_DIM // 4
    inv = ROPE_THETA ** (-jnp.arange(n_freq, dtype=jnp.float32) / n_freq)
    return jnp.concatenate([row[:, None] * inv, col[:, None] * inv], axis=-1)


def _rope_2d(x, ang):
    xp = x.reshape(*x.shape[:-1], HEAD_DIM // 2, 2)
    cos = jnp.cos(ang)[None, :, None, :].astype(x.dtype)
    sin = jnp.sin(ang)[None, :, None, :].astype(x.dtype)
    x0, x1 = xp[..., 0], xp[..., 1]
    return jnp.stack([x0 * cos - x1 * sin, x0 * sin + x1 * cos], axis=-1).reshape(x.shape)


def _attn_qkv(parts, qn_g, kn_g):
    B, L = parts['att_q'].shape[:2]
    q = _rms_norm(parts['att_q'].reshape(B, L, ATT_HEADS, HEAD_DIM), qn_g)
    k = _rms_norm(parts['att_k'].reshape(B, L, ATT_KV_HEADS, HEAD_DIM), kn_g)
    v = parts['att_v'].reshape(B, L, ATT_KV_HEADS, HEAD_DIM)
    return q, k, v


def _attend(q, k, v):
    B, Lq = q.shape[:2]
    qg = q.reshape(B, Lq, ATT_KV_HEADS, ATT_GROUP, HEAD_DIM)
    s = jnp.einsum('bqkgd,bskd->bkgqs', qg, k).astype(jnp.float32) * HEAD_DIM ** -0.5
    p = jax.nn.softmax(s, axis=-1).astype(v.dtype)
    return jnp.einsum('bkgqs,bskd->bqkgd', p, v).reshape(B, Lq, ATT_HEADS * HEAD_DIM)


def _attend_blocks(q, k, v):
    B, L = q.shape[:2]
    nb = L // Q_BLOCK
    qb = q.reshape(B, nb, Q_BLOCK, ATT_HEADS, HEAD_DIM).transpose(1, 0, 2, 3, 4)
    o = lax.map(lambda qq: _attend(qq, k, v), qb)
    return o.transpose(1, 0, 2, 3).reshape(B, L, ATT_HEADS * HEAD_DIM)


def _merge(gate_logits, b_hg, b_conv, b_att, w_out):
    B, L = gate_logits.shape[:2]
    g = jax.nn.sigmoid(gate_logits).reshape(B, L, N_BRANCH, D_MODEL)
    return (g[:, :, 0] * b_hg + g[:, :, 1] * b_conv + g[:, :, 2] * b_att) @ w_out


def _peer(h, wq, k1, k2, u_tab, v_tab):
    shape = h.shape
    hb = h.reshape(-1, PEER_BLOCK, D_MODEL)
    def block(t):
        q = (t @ wq).reshape(PEER_BLOCK, PEER_HEADS, 2, PEER_DQ // 2)
        s1 = jnp.einsum('thd,nd->thn', q[:, :, 0], k1).astype(jnp.float32)
        s2 = jnp.einsum('thd,nd->thn', q[:, :, 1], k2).astype(jnp.float32)
        v1, i1 = lax.top_k(s1, PEER_TOPK)
        v2, i2 = lax.top_k(s2, PEER_TOPK)
        cand = (v1[..., :, None] + v2[..., None, :]).reshape(PEER_BLOCK, PEER_HEADS, PEER_TOPK * PEER_TOPK)
        score, ci = lax.top_k(cand, PEER_TOPK)
        expert = (jnp.take_along_axis(i1, ci // PEER_TOPK, axis=-1) * PEER_NKEYS
                  + jnp.take_along_axis(i2, ci % PEER_TOPK, axis=-1))
        g = jax.nn.softmax(score, axis=-1).astype(t.dtype)
        act = jax.nn.gelu(jnp.einsum('thkd,td->thk', u_tab[expert], t))
        return jnp.einsum('thk,thkd->td', g * act, v_tab[expert]).astype(t.dtype)
    return lax.map(block, hb).reshape(shape)


def setup_inputs(seed: int = 0) -> dict:
    key = jax.random.key(seed)
    ks = iter(jax.random.split(key, 28))
    D = D_MODEL
    def nrm(shape, scale):
        return jax.random.normal(next(ks), shape, jnp.float32) * scale
    return {
        'x': nrm((BATCH, SEQ, D), 1.0),
        'c': nrm((BATCH, D), 1.0),
        'ctx': nrm((BATCH, CTX_LEN, D), 1.0),
        'c_ctx': nrm((D,), 1.0),
        'w_ada': nrm((DEPTH, D, N_ADA * D), D ** -0.5),
        'b_ada': nrm((DEPTH, N_ADA * D), 0.02),
        'w_in': nrm((DEPTH, D, IN_WIDTH), D ** -0.5),
        'hg_lb_logits': nrm((DEPTH, 2, HG_HEADS * HG_DK), 1.0),
        'hg_norm_g': 1.0 + nrm((DEPTH, HG_DV), 0.02),
        'w_hg_o': nrm((DEPTH, HG_HEADS * HG_DV, D), (HG_HEADS * HG_DV) ** -0.5),
        'conv_dw': nrm((DEPTH, CONV_K, CONV_CH), CONV_K ** -0.5),
        'conv_b': nrm((DEPTH, CONV_CH), 0.02),
        'conv_ln_g': 1.0 + nrm((DEPTH, CONV_CH), 0.02),
        'conv_ln_b': nrm((DEPTH, CONV_CH), 0.02),
        'w_conv_o': nrm((DEPTH, CONV_CH, D), CONV_CH ** -0.5),
        'att_qn_g': 1.0 + nrm((DEPTH, HEAD_DIM), 0.02),
        'att_kn_g': 1.0 + nrm((DEPTH, HEAD_DIM), 0.02),
        'w_att_o': nrm((DEPTH, ATT_HEADS * HEAD_DIM, D), (ATT_HEADS * HEAD_DIM) ** -0.5),
        'w_out': nrm((DEPTH, D, D), D ** -0.5 * BETA),
        'ln1_g': 1.0 + nrm((DEPTH, D), 0.02),
        'ln1_b': nrm((DEPTH, D), 0.02),
        'peer_wq': nrm((DEPTH, D, PEER_HEADS * PEER_DQ), D ** -0.5),
        'peer_k1': nrm((DEPTH, PEER_NKEYS, PEER_DQ // 2), (PEER_DQ // 2) ** -0.5),
        'peer_k2': nrm((DEPTH, PEER_NKEYS, PEER_DQ // 2), (PEER_DQ // 2) ** -0.5),
        'peer_u': nrm((DEPTH, PEER_EXPERTS, D), D ** -0.5),
        'peer_v': nrm((DEPTH, PEER_EXPERTS, D), BETA * PEER_HEADS ** -0.5),
        'ln2_g': 1.0 + nrm((DEPTH, D), 0.02),
        'ln2_b': nrm((DEPTH, D), 0.02),
    }


def reference(x, c, ctx, c_ctx, w_ada, b_ada, w_in, hg_lb_logits, hg_norm_g, w_hg_o,
              conv_dw, conv_b, conv_ln_g, conv_ln_b, w_conv_o, att_qn_g, att_kn_g, w_att_o,
              w_out, ln1_g, ln1_b, peer_wq, peer_k1, peer_k2, peer_u, peer_v, ln2_g, ln2_b):
    B, L, _ = x.shape
    ang = _grid_angles(L)
    lb_p = jax.nn.softmax(hg_lb_logits.astype(jnp.float32), axis=0)
    lb = jnp.cumsum(lb_p, axis=0) - lb_p
    sil_c = jax.nn.silu(c)
    sil_cc = jax.nn.silu(c_ctx)
    for l in range(DEPTH):
        last = l == DEPTH - 1
        mx = (sil_c @ w_ada[l] + b_ada[l])[:, None, :]
        mc = sil_cc @ w_ada[l] + b_ada[l]
        xsh1, xsc1, xg1, xsh2, xsc2, xg2 = jnp.split(mx, N_ADA, axis=-1)
        csh1, csc1, cg1, csh2, csc2, cg2 = jnp.split(mc, N_ADA, axis=-1)
        px = _split_in(_modulate(x, xsh1, xsc1) @ w_in[l])
        pc = _split_in(_modulate(ctx, csh1, csc1) @ w_in[l])

        zero = jnp.zeros((B, HG_HEADS, HG_DK, HG_DV), jnp.float32)
        oc_hg, st_f, st_b = _hgrn_bidir(*_hgrn_prep(pc, lb[l, 0], lb[l, 1]), zero, zero)
        ox_hg, _, _ = _hgrn_bidir(*_hgrn_prep(px, lb[l, 0], lb[l, 1]), st_f, st_b)

        qx, kx, vx = _attn_qkv(px, att_qn_g[l], att_kn_g[l])
        qc, kc, vc = _attn_qkv(pc, att_qn_g[l], att_kn_g[l])
        qx, kx = _rope_2d(qx, ang), _rope_2d(kx, ang)
        ox_att = _attend_blocks(qx, jnp.concatenate([kx, kc], axis=1), jnp.concatenate([vx, vc], axis=1))

        mix_x = _merge(px['merge_gate'],
                       _hgrn_out(ox_hg, px['hg_g'], hg_norm_g[l], w_hg_o[l]),
                       _conformer_conv(px['conv_glu'], conv_dw[l], conv_b[l], conv_ln_g[l], conv_ln_b[l], w_conv_o[l]),
                       ox_att @ w_att_o[l], w_out[l])
        x = _layer_norm(ALPHA * x + xg1 * mix_x, ln1_g[l], ln1_b[l])
        x = _layer_norm(ALPHA * x + xg2 * _peer(_modulate(x, xsh2, xsc2), peer_wq[l], peer_k1[l],
                                                  peer_k2[l], peer_u[l], peer_v[l]), ln2_g[l], ln2_b[l])

        if not last:
            mix_c = _merge(pc['merge_gate'],
                           _hgrn_out(oc_hg, pc['hg_g'], hg_norm_g[l], w_hg_o[l]),
                           _conformer_conv(pc['conv_glu'], conv_dw[l], conv_b[l], conv_ln_g[l], conv_ln_b[l], w_conv_o[l]),
                           _attend(qc, kc, vc) @ w_att_o[l], w_out[l])
            ctx = _layer_norm(ALPHA * ctx + cg1 * mix_c, ln1_g[l], ln1_b[l])
            ctx = _layer_norm(ALPHA * ctx + cg2 * _peer(_modulate(ctx, csh2, csc2), peer_wq[l], peer_k1[l],
                                                          peer_k2[l], peer_u[l], peer_v[l]), ln2_g[l], ln2_b[l])
    return x
```

```python
import numpy as np
from contextlib import ExitStack
import concourse.bass as bass
import concourse.mybir as mybir
from concourse.bass_utils import run_bass_kernel_spmd

F32 = mybir.dt.float32
F32R = mybir.dt.float32r
BF16 = mybir.dt.bfloat16
I32 = mybir.dt.int32
U32 = mybir.dt.uint32
AF = mybir.ActivationFunctionType
ALU = mybir.AluOpType
AX = mybir.AxisListType

D = 1024
NCTX = 256
NXF = 4096
NX = 2048
NT = NCTX + NX
NTILE = NT // 128
NKT = (NCTX + NXF) // 128
DEPTH = 2
INW = 12288
EPS = 1e-6
ALPHA = (2 * DEPTH) ** 0.25
CH = 16
NCH = NT // CH
C_HGQ, C_HGFF, C_HGFB, C_HGI, C_HGG, C_GLUA, C_GLUG, C_ATQ, C_ATK, C_ATV, C_MG = (
    0, 1024, 2048, 3072, 4096, 5120, 6144, 7168, 8192, 8704, 9216)

NDMASEM = 24


class Prog:
    def __init__(self, nc):
        self.nc = nc
        self.eng = {"pe": nc.tensor, "dve": nc.vector, "act": nc.scalar,
                    "pool": nc.gpsimd, "sp": nc.sync}
        self.sem = {k: nc.alloc_semaphore(name="es_" + k) for k in self.eng}
        self.cnt = {k: 0 for k in self.eng}
        self.dsem = {q: [nc.alloc_semaphore(name=f"ds_{q}_{i}") for i in range(NDMASEM)]
                     for q in ("sp", "pool", "act")}
        self.dcnt = {q: 0 for q in self.dsem}
        self.dval = {q: [0] * NDMASEM for q in self.dsem}
        self.known = {k: {} for k in self.eng}
        self.res = {}
        self.ninstr = 0
        self.rr = 0
        self.ccsems = []
        self.ccs = []

    def _r(self, key):
        r = self.res.get(key)
        if r is None:
            r = self.res[key] = [{}, {}]
        return r

    def _need(self, e, tok):
        sh, sname, val, src = tok
        if src == e and e == "pe":
            return
        if self.known[e].get(sname, 0) >= val:
            return
        self.eng[e].wait_ge(sh, val)
        self.known[e][sname] = val
        self.ninstr += 1

    def _deps(self, e, rkeys, wkeys, is_dma=False):
        for k in rkeys:
            for tok in self._r(k)[0].values():
                self._need(e, tok)
        for k in wkeys:
            r = self._r(k)
            for tok in r[0].values():
                w_is_dma = tok[3].startswith("dma") or tok[3].startswith("cc")
                if is_dma and w_is_dma:
                    continue
                if is_dma or w_is_dma or tok[3] != e:
                    self._need(e, tok)
            for tok in r[1].values():
                if is_dma or tok[3] != e:
                    self._need(e, tok)

    def _mark(self, tok, rkeys, wkeys):
        sname = tok[1]
        is_dma = tok[3].startswith("dma") or tok[3].startswith("cc")
        for k in rkeys:
            self._r(k)[1][sname] = tok
        for k in wkeys:
            r = self._r(k)
            if is_dma:
                r[0] = {n: t for n, t in r[0].items() if t[3].startswith("dma") or t[3].startswith("cc")}
                r[0][sname] = tok
            else:
                r[0] = {sname: tok}
            r[1] = {}

    @staticmethod
    def _split(lst):
        aps, keys = [], []
        for x in lst:
            if isinstance(x, tuple):
                aps.append(x[0]); keys.append(x[1])
            elif isinstance(x, str):
                keys.append(x)
            else:
                aps.append(x); keys.append(x.name)
        return aps, keys

    def op(self, e, fn, ins, outs):
        _, rk = self._split(ins)
        _, wk = self._split(outs)
        self._deps(e, rk, wk)
        ins_ = fn()
        self.cnt[e] += 1
        ins_.then_inc(self.sem[e], 1)
        tok = (self.sem[e], "es_" + e, self.cnt[e], e)
        self._mark(tok, rk, wk)
        self.ninstr += 1
        return ins_

    def dma(self, q, out, in_, extra_in=(), **kw):
        oa, wk = self._split([out])
        ia, rk = self._split([in_] + list(extra_in))
        self._deps(q, rk, wk, is_dma=True)
        i = self.dcnt[q] % NDMASEM
        self.dcnt[q] += 1
        self.dval[q][i] += 16
        sh = self.dsem[q][i]
        ins_ = self.eng[q].dma_start(out=oa[0], in_=ia[0], **kw)
        ins_.then_inc(sh, 16)
        tok = (sh, f"ds_{q}_{i}", self.dval[q][i], f"dma_{q}_{self.dcnt[q]}")
        self._mark(tok, rk, wk)
        self.ninstr += 1
        return tok

    def gather(self, out, table, idx_ap, extra_in=()):
        q = "pool"
        oa, wk = self._split([out])
        ia, rk = self._split([table, idx_ap] + list(extra_in))
        self._deps(q, rk, wk, is_dma=True)
        i = self.dcnt[q] % NDMASEM
        self.dcnt[q] += 1
        self.dval[q][i] += 16
        sh = self.dsem[q][i]
        ins_ = self.nc.gpsimd.indirect_dma_start(
            out=oa[0], out_offset=None, in_=ia[0],
            in_offset=bass.IndirectOffsetOnAxis(ap=ia[1], axis=0))
        ins_.then_inc(sh, 16)
        tok = (sh, f"ds_{q}_{i}", self.dval[q][i], f"dma_{q}_{self.dcnt[q]}")
        self._mark(tok, rk, wk)
        self.ninstr += 1
        return tok

    def allgather_pairs(self, in_ap, out_ap):
        q = "pool"
        ia, rk = self._split([in_ap]); oa, wk = self._split([out_ap])
        self._deps(q, rk, wk, is_dma=True)
        sh = self.nc.alloc_semaphore(name=f"cc_{len(self.ccsems)}")
        self.ccsems.append(sh)
        ins_ = self.nc.gpsimd.collective_compute(
            "AllGather", ALU.bypass, replica_groups=[[0, 1], [2, 3], [4, 5], [6, 7]],
            ins=[ia[0].opt()], outs=[oa[0].opt()])
        ins_.then_inc(sh)
        tok = (sh, f"cc_{len(self.ccsems)}", 1, f"cc_{len(self.ccsems)}")
        self._mark(tok, rk, wk)
        self.ccs.append(tok)
        self.ninstr += 1
        return tok

    def ldq(self):
        self.rr += 1
        return "sp"

    def barrier(self):
        e = "sp"
        for k in ("pe", "dve", "act", "pool"):
            if self.cnt[k]:
                self._need(e, (self.sem[k], "es_" + k, self.cnt[k], k))
        for q in self.dsem:
            for i in range(NDMASEM):
                if self.dval[q][i]:
                    self._need(e, (self.dsem[q][i], f"ds_{q}_{i}", self.dval[q][i], "dma"))
        for tok in self.ccs:
            self._need(e, tok)
        ins_ = self.nc.sync.nop()
        self.cnt[e] += 1
        ins_.then_inc(self.sem[e], 1)
        tok = (self.sem[e], "es_sp", self.cnt[e], "sp")
        for k in ("pe", "dve", "act", "pool"):
            self._need(k, tok)
        self.res = {}


class B:
    def __init__(self, debug=()):
        self.nc = nc = bass.Bass("TRN2", target_bir_lowering=False)
        self.p = Prog(nc)
        self.debug = set(debug)
        self.dr = {}

    def inp(self, name, shape, dt=F32):
        self.dr[name] = self.nc.dram_tensor(name, list(shape), dt, kind="ExternalInput").ap()
        return self.dr[name]

    def outp(self, name, shape, dt=F32):
        self.dr[name] = self.nc.dram_tensor(name, list(shape), dt, kind="ExternalOutput").ap()
        return self.dr[name]

    def scr(self, name, shape, dt=F32):
        kind = "ExternalOutput" if name in self.debug else "Internal"
        self.dr[name] = self.nc.dram_tensor(name, list(shape), dt, kind=kind).ap()
        return self.dr[name]

    def mm(self, out, lhsT, rhs, start=True, stop=True):
        nc = self.nc
        return self.p.op("pe", lambda: nc.tensor.matmul(_a(out), _a(lhsT), _a(rhs), start=start, stop=stop),
                         [lhsT, rhs], [out])

    def tr(self, out, in_, ident):
        nc = self.nc
        return self.p.op("pe", lambda: nc.tensor.transpose(_a(out), _a(in_), _a(ident)), [in_, ident], [out])

    def act(self, out, in_, func, bias=None, scale=None, e="act"):
        nc = self.nc
        kw = {}
        ins = [in_]
        if bias is not None:
            kw["bias"] = _a(bias) if not isinstance(bias, (int, float)) else bias
            if not isinstance(bias, (int, float)):
                ins.append(bias)
        if scale is not None:
            kw["scale"] = _a(scale) if not isinstance(scale, (int, float)) else scale
            if not isinstance(scale, (int, float)):
                ins.append(scale)
        return self.p.op("act", lambda: nc.scalar.activation(_a(out), _a(in_), func, **kw), ins, [out])

    def tt(self, e, out, in0, in1, op):
        eng = self.p.eng[e]
        return self.p.op(e, lambda: eng.tensor_tensor(_a(out), _a(in0), _a(in1), op), [in0, in1], [out])

    def ts(self, e, out, in0, s1, s2=None, op0=ALU.mult, op1=None):
        eng = self.p.eng[e]
        ins = [in0]
        a1 = s1
        if not isinstance(s1, (int, float)):
            ins.append(s1); a1 = _a(s1)
        a2 = s2
        if s2 is not None and not isinstance(s2, (int, float)):
            ins.append(s2); a2 = _a(s2)
        if op1 is None:
            return self.p.op(e, lambda: eng.tensor_scalar(_a(out), _a(in0), a1, None, op0), ins, [out])
        return self.p.op(e, lambda: eng.tensor_scalar(_a(out), _a(in0), a1, a2, op0, op1), ins, [out])

    def stt(self, out, in0, scalar, in1, op0, op1, accum_out=None):
        nc = self.nc
        ins = [in0, in1]
        a = scalar
        if not isinstance(scalar, (int, float)):
            ins.append(scalar); a = _a(scalar)
        if accum_out is not None:
            return self.p.op("dve", lambda: nc.vector.scalar_tensor_tensor(_a(out), _a(in0), a, _a(in1), op0, op1, accum_out=_a(accum_out)),
                             ins, [out, accum_out])
        return self.p.op("dve", lambda: nc.vector.scalar_tensor_tensor(_a(out), _a(in0), a, _a(in1), op0, op1), ins, [out])

    def cp(self, e, out, in_):
        eng = self.p.eng[e]
        if e == "act":
            return self.p.op(e, lambda: eng.copy(_a(out), _a(in_)), [in_], [out])
        return self.p.op(e, lambda: eng.tensor_copy(_a(out), _a(in_)), [in_], [out])

    def memset(self, e, out, val):
        eng = self.p.eng[e]
        return self.p.op(e, lambda: eng.memset(_a(out), val), [], [out])

    def recip(self, out, in_):
        nc = self.nc
        return self.p.op("dve", lambda: nc.vector.reciprocal(_a(out), _a(in_)), [in_], [out])

    def ld(self, out, in_, q="sp", **kw):
        return self.p.dma(q, out, in_, **kw)


def _a(x):
    return x[0] if isinstance(x, tuple) else x


def K(ap, key):
    return (ap, key)


WNAMES = [("w_ada", (2, 1024, 6144)), ("b_ada", (2, 6144)), ("w_in", (2, 1024, INW)),
          ("hg_lb_logits", (2, 2, 1024)), ("hg_norm_g", (2, 128)), ("w_hg_o", (2, 1024, 1024)),
          ("conv_dw", (2, 31, 1024)), ("conv_b", (2, 1024)), ("conv_ln_g", (2, 1024)),
          ("conv_ln_b", (2, 1024)), ("w_conv_o", (2, 1024, 1024)), ("att_qn_g", (2, 128)),
          ("att_kn_g", (2, 128)), ("w_att_o", (2, 1024, 1024)), ("w_out", (2, 1024, 1024)),
          ("ln1_g", (2, 1024)), ("ln1_b", (2, 1024)), ("peer_wq", (2, 1024, 2048)),
          ("peer_k1", (2, 128, 128)), ("peer_k2", (2, 128, 128)), ("peer_u", (2, 16384, 1024)),
          ("peer_v", (2, 16384, 1024)), ("ln2_g", (2, 1024)), ("ln2_b", (2, 1024))]


def host_consts():
    c = {}
    c["ident"] = np.eye(128, dtype=np.float32)
    s = np.arange(128)[:, None]
    t = np.arange(128)[None, :]
    same = (s // CH) == (t // CH)
    c["maskf"] = (same & (t >= s)).astype(np.float32)
    c["maskb"] = (same & (t <= s)).astype(np.float32)
    n_freq = 32
    inv = (10000.0 ** (-np.arange(n_freq, dtype=np.float32) / np.float32(n_freq))).astype(np.float32)
    row = np.repeat(np.arange(64), 64).astype(np.float32)
    col = np.tile(np.arange(64), 64).astype(np.float32)
    ang = np.concatenate([row[:, None] * inv, col[:, None] * inv], axis=-1).astype(np.float32)
    c["ropecs_full"] = np.concatenate([np.cos(ang), np.sin(ang)], axis=-1).astype(np.float32)
    c["iota256"] = np.tile(np.arange(256, dtype=np.float32)[None, :], (128, 1))
    return c


DBG_SHAPES = {"DBG_A": (128, 8352), "DBG_B": (1024, NT), "DBG_P": (NT, 1024), "DBG_E": (NT, 128), "DBG_W": (NT, 128), "DBG_ACT": (NT, 128), "DBG_SC": (NT, 2048), "DBG_V1": (NT, 256), "DBG_I1": (NT, 256), "DBG_HH": (NT, 1024), "DBG_SCO": (NT, 128), "DBG_CI": (NT, 128)}
CONST_SHAPES = {"ident": (128, 128), "maskf": (128, 128), "maskb": (128, 128),
                "ropecs": (NX, 128), "iota256": (128, 256), "rolem": (128, 2)}


def build(nlayers=DEPTH, stop_after=None, debug=()):
    b = B(debug)
    nc, p, dr = b.nc, b.p, b.dr
    b.inp("xin", (NT, D))
    b.inp("cvec", (16, 128))
    for n, s in WNAMES:
        b.inp(n, s)
    for n, s in CONST_SHAPES.items():
        b.inp(n, s)
    b.outp("y", (NX, D))
    for n, shp in DBG_SHAPES.items():
        if n in b.debug:
            b.outp(n, shp, BF16 if n == "DBG_B" else F32)
    for n in ("XC", "X1"):
        b.scr(n, (NT, D))
    for n in ("VHG", "KTOKf", "KTOKb"):
        b.scr(n, (NT, D), BF16)
    for n in ("OHGf", "OHGb"):
        b.scr(n, (1024, NT))
    for n in ("QTf", "KTf", "QTb", "KTb"):
        b.scr(n, (1024, NT), BF16)
    for n in ("DLf", "DLb"):
        b.scr(n, (1024, NCH))
    for n in ("GT", "UT", "QTA", "YHG", "YCONV", "YATT"):
        b.scr(n, (1024, NT), BF16)
    b.scr("MG", (3072, NT), BF16)
    b.scr("KTA", (512, NCTX), BF16)
    b.scr("VATT", (NCTX, 512), BF16)
    b.scr("KXH", (512, NX), BF16)
    b.scr("VXH", (NX, 512), BF16)
    b.scr("KXF", (1024, NX), BF16)
    b.scr("VXF", (2 * NX, 512), BF16)
    b.scr("UE", (1024, 32), BF16)
    b.scr("UEF", (2048, 32), BF16)
    b.scr("UV16", (2 * 16384, 2048), BF16)
    b.scr("SND", (2048, 128))
    b.scr("RCV", (4096, 128))

    with ExitStack() as glob:
        def sb(name, shape, dt=F32, es=glob):
            return es.enter_context(nc.sbuf_tensor(name, list(shape), dt))

        def ps(name, shape, dt=F32, es=glob):
            return es.enter_context(nc.psum_tensor(name, list(shape), dt))

        ident = sb("ident_sb", (128, 128))
        identb = sb("identb_sb", (128, 128), BF16)
        ones32 = sb("ones32", (128, 128))
        onesb = sb("onesb", (128, 128), BF16)
        epsc = sb("epsc", (128, 1))
        rolem = sb("rolem_sb", (128, 2))
        b.ld(rolem[:], dr["rolem"])
        b.ld(ident[:], dr["ident"])
        b.cp("dve", identb[:], ident[:])
        b.memset("dve", ones32[:], 1.0)
        b.memset("dve", onesb[:], 1.0)
        b.memset("dve", epsc[:], EPS)
        MODP = sb("MODP", (128, 2, 2, 8))
        MODBC = sb("MODBC", (128, 2, 4, 1024))
        VT = sb("VT", (128, 80))
        LB = sb("LB", (128, 3, 16))
        st = dict(b=b, sb=sb, ps=ps, ident=ident, identb=identb, ones32=ones32, onesb=onesb,
                  epsc=epsc, rolem=rolem, MODP=MODP, MODBC=MODBC, VT=VT, LB=LB)
        p.barrier()
        if st.get("nph", 7) >= 7 and stop_after is None or (stop_after is not None and stop_after[1] == "phase_i"):
            phase_tab(st)
            p.barrier()
        for l in range(nlayers):
            last = l == DEPTH - 1
            src = dr["xin"] if l == 0 else dr["XC"]
            dst = dr["y"] if last else dr["XC"]
            phases = [phase_a, phase_bcd, phase_e, phase_f, phase_g, phase_h, phase_i][:st.get("nph", 7)]
            for ph in phases:
                ph(st, l, last, src, dst)
                p.barrier()
                if stop_after == (l, ph.__name__):
                    break
            else:
                continue
            break
        p.barrier()
    return b


GROUPS = [(0, 256)] + [(256 + 512 * i, 512) for i in range(NX // 512)]


def phase_a(st, l, last, src, dst):
    b = st["b"]; nc, p, dr = b.nc, b.p, b.dr
    ident, MODP, MODBC, VT, LB = st["ident"], st["MODP"], st["MODBC"], st["VT"], st["LB"]
    with ExitStack() as es:
        sb = lambda n, s, dt=F32: st["sb"](n + f"_a{l}", s, dt, es)
        ps = lambda n, s, dt=F32: st["ps"](n + f"_a{l}", s, dt, es)
        stg = sb("stg", (80, 128))
        b.ld(stg[0:16, :], dr["cvec"])
        b.ld(stg[16:48, :], dr["hg_lb_logits"].rearrange("l d (h p) -> (l d h) p", p=128))
        b.ld(stg[48:56, :], dr["conv_b"][l].rearrange("(k p) -> k p", p=128))
        b.ld(stg[56:64, :], dr["conv_ln_g"][l].rearrange("(k p) -> k p", p=128))
        b.ld(stg[64:72, :], dr["conv_ln_b"][l].rearrange("(k p) -> k p", p=128))
        b.ld(stg[72:73, :], dr["hg_norm_g"][l:l + 1, :])
        pT = ps("pT", (128, 512))
        b.tr(pT[:, 0:73], stg[0:73, :], ident[0:73, 0:73])
        b.cp("dve", VT[:, 0:73], pT[:, 0:73])
        if l == 0:
            b.memset("dve", LB[:, 0, :], 0.0)
        else:
            dlt = sb("dlt", (128, 16))
            b.tt("dve", dlt[:], VT[:, 16:32], VT[:, 32:48], ALU.subtract)
            b.act(LB[:, 0, :], dlt[:], AF.Sigmoid)
        b.ts("dve", LB[:, 1, :], LB[:, 0, :], -1.0, 1.0, ALU.mult, ALU.add)
        b.ts("dve", LB[:, 2, :], LB[:, 0, :], -1.0, None, ALU.add)
        csil = sb("csil", (128, 16))
        b.act(csil[:], VT[:, 0:16], AF.Silu)
        crep = sb("crep", (128, 16, 128))
        for j in range(16):
            b.cp("dve", K(crep[:, j, :], f"crep{j}"), csil[:, j:j + 1].to_broadcast([128, 128]))
        wad = [sb(f"wad{i}", (128, 8, 512)) for i in range(2)]
        bia = [sb(f"bia{i}", (128, 512)) for i in range(2)]
        tmp = [sb(f"tmpa{i}", (128, 512)) for i in range(2)]
        pA = [ps(f"pA{i}", (128, 512)) for i in range(2)]
        for n in range(12):
            w = wad[n % 2]; bi = bia[n % 2]
            b.ld(w[:], dr["w_ada"][l, :, n * 512:(n + 1) * 512].rearrange("(k p) c -> p k c", p=128))
            b.ld(bi[:], dr["b_ada"][l, n * 512:(n + 1) * 512].partition_broadcast(128), q="pool")
            idx6, half = n // 2, n % 2
            for v in range(2):
                for k in range(8):
                    b.mm(pA[v][:], K(crep[:, v * 8 + k, :], f"crep{v * 8 + k}"), w[:, k, :], start=(k == 0), stop=(k == 7))
                t = tmp[v]
                b.tt("dve", t[:], pA[v][:], bi[:], ALU.add)
                hs = slice(half * 512, (half + 1) * 512)
                if idx6 in (0, 1):
                    for j in range(4):
                        b.tr(pT[:, j * 128:(j + 1) * 128], t[:, j * 128:(j + 1) * 128], ident[:])
                    for j in range(4):
                        o = MODP[:, v, idx6, half * 4 + j:half * 4 + j + 1]
                        if idx6 == 0:
                            b.cp("dve", o, pT[:, j * 128:j * 128 + 1])
                        else:
                            b.ts("dve", o, pT[:, j * 128:j * 128 + 1], 1.0, None, ALU.add)
                elif idx6 == 2:
                    b.cp("act", MODBC[:, v, 0, hs], t[:])
                elif idx6 == 3:
                    b.cp("act", MODBC[:, v, 2, hs], t[:])
                elif idx6 == 4:
                    b.ts("dve", MODBC[:, v, 3, hs], t[:], 1.0, None, ALU.add)
                else:
                    b.cp("act", MODBC[:, v, 1, hs], t[:])
        if "DBG_A" in b.debug:
            b.ld(dr["DBG_A"][:, 0:8192], MODBC[:].rearrange("p a b c -> p (a b c)"), q="pool")
            b.ld(dr["DBG_A"][:, 8192:8224], MODP[:].rearrange("p a b c -> p (a b c)"), q="pool")
            b.ld(dr["DBG_A"][:, 8224:8304], VT[:], q="pool")
            b.ld(dr["DBG_A"][:, 8304:8352], LB[:].rearrange("p a b -> p (a b)"), q="pool")
        p.barrier()


def phase_bcd(st, l, last, src, dst):
    b = st["b"]; nc, p, dr = b.nc, b.p, b.dr
    ident, identb, MODP, LB = st["ident"], st["identb"], st["MODP"], st["LB"]
    with ExitStack() as es0:
        xmodT = st["sb"](f"xmodT{l}", (128, 8, NT), BF16, es0)
        with ExitStack() as es:
            sb = lambda n, s, dt=F32: st["sb"](n + f"_b{l}", s, dt, es)
            ps = lambda n, s, dt=F32: st["ps"](n + f"_b{l}", s, dt, es)
            xt = [sb(f"xt{i}", (128, D)) for i in range(3)]
            pB = [ps(f"pB{i}", (128, 512)) for i in range(4)]
            cnt = 0
            for i in range(NTILE):
                v = 1 if i < 2 else 0
                x_ = xt[i % 3]
                b.ld(x_[:], src[i * 128:(i + 1) * 128, :])
                for hf in range(2):
                    pb = pB[(2 * i + hf) % 4]
                    for j in range(4):
                        k = hf * 4 + j
                        b.tr(pb[:, j * 128:(j + 1) * 128], x_[:, k * 128:(k + 1) * 128], ident[:])
                    for j in range(4):
                        k = hf * 4 + j
                        o = K(xmodT[:, k, i * 128:(i + 1) * 128], f"xmodT:{i}")
                        if cnt % 2 == 0:
                            b.act(o, pb[:, j * 128:(j + 1) * 128], AF.Identity,
                                  bias=MODP[:, v, 0, k:k + 1], scale=MODP[:, v, 1, k:k + 1])
                        else:
                            b.ts("dve", o, pb[:, j * 128:(j + 1) * 128], MODP[:, v, 1, k:k + 1],
                                 MODP[:, v, 0, k:k + 1], ALU.mult, ALU.add)
                        cnt += 1
            if "DBG_B" in b.debug:
                b.ld(dr["DBG_B"].rearrange("(k p) t -> p k t", p=128), xmodT[:], q="pool")
            p.barrier()
        xm = "xmodT_ro"

        def proj_fm(pst, wbf, t0, n):
            for k in range(8):
                b.mm(pst[:, 0:n], wbf[:, k, :], K(xmodT[:, k, t0:t0 + n], xm), start=(k == 0), stop=(k == 7))

        with ExitStack() as es:
            sb = lambda n, s, dt=F32: st["sb"](n + f"_c{l}", s, dt, es)
            ps = lambda n, s, dt=F32: st["ps"](n + f"_c{l}", s, dt, es)
            wtmp = [sb(f"wtmp{i}", (128, 8, 128)) for i in range(3)]
            wbf = [sb(f"wbf{i}", (128, 8, 128), BF16) for i in range(6)]
            wc = [0]

            def loadw(col0):
                i = wc[0]; wc[0] += 1
                wt = wtmp[i % 3]; wb = wbf[i % 6]
                b.ld(wt[:], dr["w_in"][l, :, col0:col0 + 128].rearrange("(k p) c -> p k c", p=128))
                b.cp("pool", wb[:], wt[:])
                return wb

            pC = [ps(f"pC{i}", (128, 512)) for i in range(6)]
            pT = [ps(f"pT{i}", (128, 1024), BF16) for i in range(2)]
            qf = [sb(f"qf{i}", (128, 512)) for i in range(2)]
            tb = {}
            for d in range(2):
                for nm in ("sg", "lf", "bp", "kk", "tm", "E", "Ei"):
                    tb[nm, d] = sb(f"{nm}{d}", (128, 512))
                for nm in ("qt", "kt", "kh"):
                    for par in range(2):
                        tb[nm, d, par] = sb(f"{nm}{d}{par}", (128, 512), BF16)
                for par in range(2):
                    tb["ktok", d, par] = sb(f"ktok{d}{par}", (128, 4, 128), BF16)
                    tb["dl", d, par] = sb(f"dl{d}{par}", (128, 32))
            jobs = [(h, t0, n) for h in range(8) for (t0, n) in GROUPS]
            wts = {}

            def proj_job(ji):
                h, t0, n = jobs[ji]
                if h not in wts:
                    wts[h] = (loadw(C_HGQ + h * 128), loadw(C_HGFF + h * 128), loadw(C_HGFB + h * 128))
                par = ji % 2
                proj_fm(pC[3 * par], wts[h][0], t0, n)
                proj_fm(pC[3 * par + 1], wts[h][1], t0, n)
                proj_fm(pC[3 * par + 2], wts[h][2], t0, n)

            def chain1(ji, d):
                h, t0, n = jobs[ji]
                par = ji % 2; nck = n // CH
                pz = pC[3 * par + 1 + d]
                q_ = qf[par]
                sg, lf, bp, kk, tm, E, Ei = (tb[nm, d] for nm in ("sg", "lf", "bp", "kk", "tm", "E", "Ei"))
                qt, kt, kh, ktok, dl = (tb[nm, d, par] for nm in ("qt", "kt", "kh", "ktok", "dl"))
                ci = d * 8 + h
                b.act(sg[:, 0:n], pz[:, 0:n], AF.Sigmoid); yield
                b.ts("pool", kk[:, 0:n], sg[:, 0:n], LB[:, 2, ci:ci + 1], LB[:, 1, ci:ci + 1], ALU.mult, ALU.add); yield
                b.ts("dve", tm[:, 0:n], sg[:, 0:n], LB[:, 1, ci:ci + 1], LB[:, 0, ci:ci + 1], ALU.mult, ALU.add); yield
                b.act(lf[:, 0:n], tm[:, 0:n], AF.Ln); yield
                srcb = lf
                for si, sh_ in enumerate((1, 2, 4, 8)):
                    dstb = tm if si % 2 == 0 else bp
                    sv = srcb[:, 0:n].rearrange("p (c s) -> p c s", s=CH)
                    dv = dstb[:, 0:n].rearrange("p (c s) -> p c s", s=CH)
                    b.tt("dve", dv[:, :, sh_:], sv[:, :, sh_:], sv[:, :, :CH - sh_], ALU.add); yield
                    b.cp("pool", dv[:, :, 0:sh_], sv[:, :, 0:sh_]); yield
                    srcb = dstb
                bl = bp[:, CH - 1:n:CH]
                if d == 0:
                    bb = bp
                else:
                    b.tt("dve", tm[:, 0:n], lf[:, 0:n], bp[:, 0:n], ALU.subtract); yield
                    b.tt("dve", tm[:, 0:n].rearrange("p (c s) -> p c s", s=CH),
                         tm[:, 0:n].rearrange("p (c s) -> p c s", s=CH),
                         bp[:, 0:n].rearrange("p (c s) -> p c s", s=CH)[:, :, CH - 1:CH].to_broadcast([128, nck, CH]),
                         ALU.add); yield
                    bb = tm
                b.act(dl[:, 0:nck], bl, AF.Exp); yield
                b.ld(dr["DLf" if d == 0 else "DLb"][h * 128:(h + 1) * 128, t0 // CH:t0 // CH + nck], dl[:, 0:nck], q="pool")
                b.ts("dve", E[:, 0:n], bb[:, 0:n], -80.0, None, ALU.max); yield
                b.act(Ei[:, 0:n], E[:, 0:n], AF.Exp, scale=-1.0); yield
                b.act(E[:, 0:n], E[:, 0:n], AF.Exp); yield
                b.tt("dve", qt[:, 0:n], q_[:, 0:n], E[:, 0:n], ALU.mult); yield
                b.tt("pool", kt[:, 0:n], kk[:, 0:n], Ei[:, 0:n], ALU.mult); yield
                dn = "f" if d == 0 else "b"
                b.ld(dr["QT" + dn][h * 128:(h + 1) * 128, t0:t0 + n], qt[:, 0:n], q="pool")
                b.ld(dr["KT" + dn][h * 128:(h + 1) * 128, t0:t0 + n], kt[:, 0:n], q="pool")
                b.tt("dve", lf[:, 0:n].rearrange("p (c s) -> p c s", s=CH),
                     bp[:, 0:n].rearrange("p (c s) -> p c s", s=CH)[:, :, CH - 1:CH].to_broadcast([128, nck, CH]),
                     bb[:, 0:n].rearrange("p (c s) -> p c s", s=CH), ALU.subtract); yield
                b.act(lf[:, 0:n], lf[:, 0:n], AF.Exp); yield
                b.tt("pool", kh[:, 0:n], kk[:, 0:n], lf[:, 0:n], ALU.mult); yield

            def chain2(ji, d):
                h, t0, n = jobs[ji]
                par = ji % 2
                kh, ktok = tb["kh", d, par], tb["ktok", d, par]
                dn = "f" if d == 0 else "b"
                pt = pT[d]
                for j in range(n // 128):
                    b.tr(pt[:, j * 128:(j + 1) * 128], kh[:, j * 128:(j + 1) * 128], identb[:])
                b.cp("act", ktok[:, 0:n // 128, :], pt[:, 0:n].rearrange("p (j c) -> p j c", c=128))
                b.ld(dr["KTOK" + dn][t0:t0 + n, h * 128:(h + 1) * 128].rearrange("(j p) c -> p j c", p=128),
                     ktok[:, 0:n // 128, :], q="pool")

            proj_job(0)
            for ji, (h, t0, n) in enumerate(jobs):
                par = ji % 2
                b.act(qf[par][:, 0:n], pC[3 * par][:, 0:n], AF.Silu)
                gens = [chain1(ji, 0), chain1(ji, 1)]
                while gens:
                    for g_ in list(gens):
                        if next(g_, "done") == "done":
                            gens.remove(g_)
                if ji + 1 < len(jobs):
                    proj_job(ji + 1)
                chain2(ji, 0); chain2(ji, 1)
            job = len(jobs)
            ob = [sb(f"ob{i}", (128, 512), BF16) for i in range(3)]
            sgl = [sb(f"sgl{i}", (128, 512)) for i in range(2)]
            oc = 0
            for c in range(8):
                wg = loadw(C_HGG + c * 128)
                wa = loadw(C_GLUA + c * 128)
                wgg = loadw(C_GLUG + c * 128)
                for (t0, n) in GROUPS:
                    par = job % 2; job += 1
                    pg, pa, pgg = pC[3 * par], pC[3 * par + 1], pC[3 * par + 2]
                    proj_fm(pg, wg, t0, n)
                    proj_fm(pa, wa, t0, n)
                    proj_fm(pgg, wgg, t0, n)
                    o = ob[oc % 3]; oc += 1
                    b.act(o[:, 0:n], pg[:, 0:n], AF.Silu)
                    b.ld(dr["GT"][c * 128:(c + 1) * 128, t0:t0 + n], o[:, 0:n], q="pool")
                    s_ = sgl[par]
                    b.act(s_[:, 0:n], pgg[:, 0:n], AF.Sigmoid)
                    o = ob[oc % 3]; oc += 1
                    b.tt("dve", o[:, 0:n], pa[:, 0:n], s_[:, 0:n], ALU.mult)
                    b.ld(dr["UT"][c * 128:(c + 1) * 128, t0:t0 + n], o[:, 0:n], q="pool")
            for c in range(24):
                wm = loadw(C_MG + c * 128)
                for (t0, n) in GROUPS:
                    pm = pC[job % 6]; job += 1
                    proj_fm(pm, wm, t0, n)
                    o = ob[oc % 3]; oc += 1
                    b.act(o[:, 0:n], pm[:, 0:n], AF.Sigmoid)
                    b.ld(dr["MG"][c * 128:(c + 1) * 128, t0:t0 + n], o[:, 0:n], q="pool")
            p.barrier()
        if st.get("skip_d"):
            return
        phase_d(st, l, last, xmodT, xm)


def phase_d(st, l, last, xmodT, xm):
    b = st["b"]; nc, p, dr = b.nc, b.p, b.dr
    ident, identb = st["ident"], st["identb"]
    with ExitStack() as es:
        sb = lambda n, s, dt=F32: st["sb"](n + f"_d{l}", s, dt, es)
        ps = lambda n, s, dt=F32: st["ps"](n + f"_d{l}", s, dt, es)
        WD = sb("WD", (128, 8, 3072), BF16)
        wst = [sb(f"wst{i}", (128, 8, 256)) for i in range(1)]
        cols = [C_HGI, C_HGI + 512, C_ATQ, C_ATQ + 512, C_ATK, C_ATV]
        for n_, c0 in enumerate(cols):
            for hh in range(2):
                w = wst[0]
                b.ld(w[:], dr["w_in"][l, :, c0 + hh * 256:c0 + hh * 256 + 256].rearrange("(k p) c -> p k c", p=128))
                b.cp("pool", K(WD[:, :, n_ * 512 + hh * 256:n_ * 512 + hh * 256 + 256], f"WD{n_}"), w[:])
        gq = sb("gq", (128, 128)); gk = sb("gk", (128, 128))
        b.ld(gq[:], dr["att_qn_g"][l].partition_broadcast(128), q="pool")
        b.ld(gk[:], dr["att_kn_g"][l].partition_broadcast(128), q="pool")
        pD = [ps(f"pD{i}", (128, 512)) for i in range(6)]
        pTq = ps("pTq", (128, 1024), BF16)
        pTk = ps("pTk", (128, 1024), BF16)
        vst = [sb(f"vst{i}", (128, 1024), BF16) for i in range(2)]
        vat = [sb(f"vat{i}", (128, 512), BF16) for i in range(2)]
        junk = sb("junk", (128, 128))
        ssq = [sb(f"ssq{i}", (128, 12)) for i in range(2)]
        qn = [sb(f"qn{i}", (128, 12, 128)) for i in range(1)] * 2
        qr = [sb(f"qr{i}", (128, 12, 128), BF16) for i in range(2)]
        cs = [sb(f"cs{i}", (128, 128)) for i in range(2)]
        rt = [sb(f"rt{i}", (128, 12, 64)) for i in range(4)]
        qTs = [sb(f"qTs{i}", (128, 8, 128), BF16) for i in range(2)]
        kTs = [sb(f"kTs{i}", (128, 4, 128), BF16) for i in range(2)]
        def mm_tile(i):
            t0 = i * 128
            for n_ in range(6):
                for k in range(8):
                    b.mm(pD[n_][:], K(xmodT[:, k, t0:t0 + 128], xm), K(WD[:, k, n_ * 512:(n_ + 1) * 512], f"WD{n_}"),
                         start=(k == 0), stop=(k == 7))

        mm_tile(0)
        for i in range(NTILE):
            par = i % 2
            t0 = i * 128
            v_ = vst[par]
            b.cp("act", v_[:, 0:512], pD[0][:])
            b.cp("act", v_[:, 512:1024], pD[1][:])
            b.ld(dr["VHG"][t0:t0 + 128, :], v_[:], q="pool")
            va = vat[par]
            b.cp("act", va[:], pD[5][:])
            if i < 2:
                b.ld(dr["VATT"][t0:t0 + 128, :], va[:], q="pool")
            else:
                b.ld(dr["VXH"][t0 - NCTX:t0 - NCTX + 128, :], va[:], q="pool")
            s_ = ssq[par]
            q_ = qn[par]
            for jj in range(3):
                b.cp("act", q_[:, jj * 4:(jj + 1) * 4, :], pD[2 + jj][:].rearrange("p (h c) -> p h c", c=128))
            hp = lambda j: q_[:, j, :]
            for j in range(12):
                b.stt(junk[:], hp(j), 1.0, hp(j), ALU.mult, ALU.mult, accum_out=s_[:, j:j + 1])
            b.ts("dve", s_[:], s_[:], 1.0 / 128.0, EPS, ALU.mult, ALU.add)
            b.act(s_[:], s_[:], AF.Sqrt)
            b.recip(s_[:], s_[:])
            for j in range(12):
                b.stt(q_[:, j, :], hp(j), s_[:, j:j + 1], (gq if j < 8 else gk)[:], ALU.mult, ALU.mult)
            r_ = qr[par]
            if i >= 2:
                c_ = cs[par]
                b.ld(c_[:], dr["ropecs"][(i - 2) * 128:(i - 1) * 128, :])
                x0 = q_[:, :, 0:128:2]; x1 = q_[:, :, 1:128:2]
                cosb = c_[:, None, 0:64].to_broadcast([128, 12, 64])
                sinb = c_[:, None, 64:128].to_broadcast([128, 12, 64])
                t1, t2, t3, t4 = rt
                b.tt("dve", t1[:], x0, cosb, ALU.mult)
                b.tt("pool", t2[:], x1, sinb, ALU.mult)
                b.tt("dve", t3[:], x0, sinb, ALU.mult)
                b.tt("pool", t4[:], x1, cosb, ALU.mult)
                b.tt("dve", r_[:, :, 0:128:2], t1[:], t2[:], ALU.subtract)
                b.tt("pool", r_[:, :, 1:128:2], t3[:], t4[:], ALU.add)
            else:
                b.cp("dve", r_[:], q_[:])
            if i + 1 < NTILE:
                mm_tile(i + 1)
            for j in range(8):
                b.tr(pTq[:, j * 128:(j + 1) * 128], r_[:, j, :], identb[:])
            for j in range(4):
                b.tr(pTk[:, j * 128:(j + 1) * 128], r_[:, 8 + j, :], identb[:])
            qs, ks = qTs[par], kTs[par]
            b.cp("act", qs[:], pTq[:].rearrange("p (h t) -> p h t", t=128))
            b.cp("dve", ks[:], pTk[:, 0:512].rearrange("p (h t) -> p h t", t=128))
            b.ld(dr["QTA"][:, t0:t0 + 128].rearrange("(h p) t -> p h t", p=128), qs[:], q="pool")
            if i < 2:
                b.ld(dr["KTA"][:, t0:t0 + 128].rearrange("(h p) t -> p h t", p=128), ks[:], q="pool")
            else:
                b.ld(dr["KXH"][:, t0 - NCTX:t0 - NCTX + 128].rearrange("(h p) t -> p h t", p=128), ks[:], q="pool")
        ues = sb("ues", (128, 8, 32), BF16)
        b.memset("pool", ues[:], 0.0)
        b.ld(ues[:, :, 0:15], dr["UT"][:, NCTX:NCTX + 15].rearrange("(c p) t -> p c t", p=128))
        b.ld(ues[:, :, 16:31], dr["UT"][:, NT - 15:NT].rearrange("(c p) t -> p c t", p=128))
        b.ld(dr["UE"].rearrange("(c p) t -> p c t", p=128), ues[:], q="pool")
        p.barrier()
        p.allgather_pairs(dr["KXH"], dr["KXF"])
        p.allgather_pairs(dr["VXH"], dr["VXF"])
        p.allgather_pairs(dr["UE"], dr["UEF"])
        p.barrier()


def phase_e(st, l, last, src, dst):
    b = st["b"]; nc, p, dr = b.nc, b.p, b.dr
    ident, ones32, VT, epsc = st["ident"], st["ones32"], st["VT"], st["epsc"]
    eso = ExitStack()
    SCT = [st["sb"](f"SCT{d}_{l}", (128, 8, 128), F32, eso) for d in range(2)]
    with ExitStack() as es:
        sb = lambda n, s, dt=F32: st["sb"](n + f"_e{l}", s, dt, es)
        ps = lambda n, s, dt=F32: st["ps"](n + f"_e{l}", s, dt, es)
        mask = [sb("maskf", (128, 128)), sb("maskb", (128, 128))]
        b.ld(mask[0][:], dr["maskf"]); b.ld(mask[1][:], dr["maskb"])
        S32 = [sb(f"S32_{d}", (128, 8, 128)) for d in range(2)]
        S16 = [sb(f"S16_{d}", (128, 8, 128), BF16) for d in range(2)]
        for d in range(2):
            b.memset("pool", S32[d][:], 0.0)
            b.memset("pool", S16[d][:], 0.0)
        Sk = lambda d, h: K(S32[d][:, h, :], f"S{d}{h}")
        Tk = lambda d, h: K(S16[d][:, h, :], f"T{d}{h}")
        qT = [[sb(f"qT{d}{i}", (128, 8, 128), BF16) for i in range(2)] for d in range(2)]
        kT = [[sb(f"kT{d}{i}", (128, 8, 128), BF16) for i in range(2)] for d in range(2)]
        kk = [[sb(f"kk{d}{i}", (128, 3, 1024), BF16) for i in range(2)] for d in range(2)]
        vv = [[sb(f"vv{d}{i}", (128, 3, 1024), BF16) for i in range(2)] for d in range(2)]
        vf = [[sb(f"vf{d}{i}", (128, 1024), BF16) for i in range(2)] for d in range(2)]
        dl = [[sb(f"dl{d}{i}", (128, 8, 8)) for i in range(2)] for d in range(2)]
        scm = [sb(f"scm{i}", (128, 128), BF16) for i in range(4)]
        ost2 = [[sb(f"ost{d}{i}", (128, 8, 128)) for i in range(2)] for d in range(2)]
        pO = [ps(f"pO{i}", (128, 1024)) for i in range(2)]
        pSt = [ps(f"pSt{i}", (128, 512)) for i in range(4)]
        pS = pSt[2:4]
        order = [list(range(NTILE)), [1, 0] + list(range(NTILE - 1, 1, -1))]
        nsc = 0; nst = 0
        for step in range(NTILE):
            par = step % 2
            for d in range(2):
                dn = "f" if d == 0 else "b"
                ti = order[d][step]; t0 = ti * 128
                b.ld(qT[d][par][:], dr["QT" + dn][:, t0:t0 + 128].rearrange("(h p) t -> p h t", p=128))
                b.ld(kT[d][par][:], dr["KT" + dn][:, t0:t0 + 128].rearrange("(h p) t -> p h t", p=128))
                b.ld(vf[d][par][:], dr["VHG"][t0:t0 + 128, :])
                b.ld(dl[d][par][:], dr["DL" + dn][:, t0 // CH:t0 // CH + 8].rearrange("(h p) c -> p h c", p=128))
                for j in range(8):
                    g, a = j % 3, j // 3
                    b.ld(K(kk[d][par][32 * g:32 * g + 16, a, :], kk[d][par].name), dr["KTOK" + dn][t0 + j * CH:t0 + (j + 1) * CH, :])
                    b.ld(K(vv[d][par][32 * g:32 * g + 16, a, :], vv[d][par].name), dr["VHG"][t0 + j * CH:t0 + (j + 1) * CH, :])
            ost = [ost2[0][par], ost2[1][par]]
            dh = [(d, h) for d in range(2) for h in range(8)]
            b.mm(pS[nsc % 2][:, 0:128], kT[0][par][:, 0, :], qT[0][par][:, 0, :])
            for ii, (d, h) in enumerate(dh):
                pq = pS[nsc % 2]; sm = scm[nsc % 4]; nsc += 1
                if ii + 1 < len(dh):
                    d2, h2 = dh[ii + 1]
                    b.mm(pS[nsc % 2][:, 0:128], kT[d2][par][:, h2, :], qT[d2][par][:, h2, :])
                b.tt("dve", sm[:], pq[:, 0:128], mask[d][:], ALU.mult)
                b.mm(K(pO[d][:, h * 128:(h + 1) * 128], f"pO{d}"), vf[d][par][:, h * 128:(h + 1) * 128], sm[:],
                     start=(h % 4 == 0), stop=False)
            for jj in range(8):
                for d in range(2):
                    j = jj if d == 0 else 7 - jj
                    g, a = j % 3, j // 3
                    for h in range(8):
                        b.mm(K(pO[d][:, h * 128 + j * CH:h * 128 + (j + 1) * CH], f"pO{d}"), Tk(d, h),
                             qT[d][par][:, h, j * CH:(j + 1) * CH], start=False, stop=True)
                        pt = pSt[nst % 4]; nst += 1
                        b.mm(pt[:, 0:128], kk[d][par][32 * g:32 * g + 16, a, h * 128:(h + 1) * 128],
                             vv[d][par][32 * g:32 * g + 16, a, h * 128:(h + 1) * 128], start=True, stop=True)
                        b.stt(Sk(d, h), Sk(d, h), dl[d][par][:, h, j:j + 1], pt[:, 0:128], ALU.mult, ALU.add)
                        b.act(Tk(d, h), Sk(d, h), AF.Copy)
            for d in range(2):
                b.cp("act" if d == 0 else "dve", ost[d][:], K(pO[d][:].rearrange("p (h t) -> p h t", t=128), f"pO{d}"))
            if step == 1:
                for d in range(2):
                    for h in range(8):
                        b.cp("pool", K(SCT[d][:, h, :], f"SCT{d}"), Sk(d, h))
            for d in range(2):
                dn = "f" if d == 0 else "b"
                ti = order[d][step]; t0 = ti * 128
                b.ld(dr["OHG" + dn][:, t0:t0 + 128].rearrange("(h p) t -> p h t", p=128), ost[d][:], q="pool")
        for d in range(2):
            b.p.dma("sp", dr["SND"][d * 1024:(d + 1) * 1024, :].rearrange("(h p) v -> p h v", p=128), S32[d][:],
                    extra_in=[f"S{d}{h}" for h in range(8)])
        p.barrier()
        p.allgather_pairs(dr["SND"], dr["RCV"])
        p.barrier()
    with ExitStack() as es:
        sb = lambda n, s, dt=F32: st["sb"](n + f"_e2{l}", s, dt, es)
        ps = lambda n, s, dt=F32: st["ps"](n + f"_e2{l}", s, dt, es)
        rolem = st["rolem"]
        DLT = [sb(f"DLT{d}", (128, 8, 128)) for d in range(2)]
        rows = [slice(0, 1024), slice(3072, 4096)]
        for d in range(2):
            b.ld(DLT[d][:], dr["RCV"][rows[d], :].rearrange("(h p) v -> p h v", p=128))
            b.tt("dve", DLT[d][:], DLT[d][:], K(SCT[d][:], f"SCT{d}"), ALU.subtract)
            b.ts("dve", DLT[d][:], DLT[d][:], rolem[:, d:d + 1], None, ALU.mult)
        NXC = NX // CH
        PC = [sb(f"PC{d}", (128, 8, NXC)) for d in range(2)]
        pa = sb("pca", (128, 8, NXC)); pb_ = sb("pcb", (128, 8, NXC))
        for d in range(2):
            dn = "f" if d == 0 else "b"
            b.ld(pa[:], dr["DL" + dn][:, NCTX // CH:NCH].rearrange("(h p) c -> p h c", p=128))
            srcb, dstb = pa, pb_
            sh_ = 1
            while sh_ < NXC:
                if d == 0:
                    b.tt("dve", dstb[:, :, sh_:], srcb[:, :, sh_:], srcb[:, :, :NXC - sh_], ALU.mult)
                    b.cp("pool", dstb[:, :, 0:sh_], srcb[:, :, 0:sh_])
                else:
                    b.tt("dve", dstb[:, :, :NXC - sh_], srcb[:, :, :NXC - sh_], srcb[:, :, sh_:], ALU.mult)
                    b.cp("pool", dstb[:, :, NXC - sh_:], srcb[:, :, NXC - sh_:])
                srcb, dstb = dstb, srcb
                sh_ *= 2
            if d == 0:
                b.cp("dve", PC[d][:, :, 1:], srcb[:, :, :NXC - 1])
                b.memset("pool", PC[d][:, :, 0:1], 1.0)
            else:
                b.cp("dve", PC[d][:, :, :NXC - 1], srcb[:, :, 1:])
                b.memset("pool", PC[d][:, :, NXC - 1:], 1.0)
        of = [sb(f"of{i}", (128, 512)) for i in range(2)]
        ob = [sb(f"ob{i}", (128, 512)) for i in range(2)]
        qc = [[sb(f"qc{d}{i}", (128, 512)) for i in range(2)] for d in range(2)]
        qcb = [[sb(f"qcb{d}{i}", (128, 512), BF16) for i in range(2)] for d in range(2)]
        gt = [sb(f"gt{i}", (128, 512), BF16) for i in range(2)]
        sq = [sb(f"sq{i}", (128, 512)) for i in range(2)]
        rs = [sb(f"rs{i}", (128, 512)) for i in range(2)]
        yo = [sb(f"yo{i}", (128, 512), BF16) for i in range(2)]
        pss = [ps(f"pss{i}", (128, 512)) for i in range(2)]
        pcr = [ps(f"pcr{i}", (128, 512)) for i in range(2)]
        job = 0
        for h in range(8):
            for (t0, n) in GROUPS:
                par = job % 2; job += 1
                rows_ = slice(h * 128, (h + 1) * 128)
                b.ld(of[par][:, 0:n], dr["OHGf"][rows_, t0:t0 + n])
                b.ld(ob[par][:, 0:n], dr["OHGb"][rows_, t0:t0 + n])
                b.ld(gt[par][:, 0:n], dr["GT"][rows_, t0:t0 + n])
                o = of[par]
                b.tt("dve", o[:, 0:n], of[par][:, 0:n], ob[par][:, 0:n], ALU.add)
                if t0 >= NCTX:
                    c0 = (t0 - NCTX) // CH
                    for d in range(2):
                        dn = "f" if d == 0 else "b"
                        q_ = qc[d][par]; qb_ = qcb[d][par]
                        b.ld(qb_[:, 0:n], dr["QT" + dn][rows_, t0:t0 + n])
                        qv = q_[:, 0:n].rearrange("p (c s) -> p c s", s=CH)
                        b.tt("pool", qv, qb_[:, 0:n].rearrange("p (c s) -> p c s", s=CH),
                             PC[d][:, h, c0:c0 + n // CH, None].to_broadcast([128, n // CH, CH]), ALU.mult)
                        b.mm(pcr[par][:, 0:n], DLT[d][:, h, :], q_[:, 0:n], start=(d == 0), stop=(d == 1))
                    b.tt("dve", o[:, 0:n], o[:, 0:n], pcr[par][:, 0:n], ALU.add)
                b.act(sq[par][:, 0:n], o[:, 0:n], AF.Square)
                b.mm(pss[par][:, 0:n], ones32[:], sq[par][:, 0:n])
                b.ts("dve", rs[par][:, 0:n], pss[par][:, 0:n], 1.0 / 128.0, EPS, ALU.mult, ALU.add)
                b.act(rs[par][:, 0:n], rs[par][:, 0:n], AF.Sqrt)
                b.recip(rs[par][:, 0:n], rs[par][:, 0:n])
                b.tt("dve", o[:, 0:n], o[:, 0:n], rs[par][:, 0:n], ALU.mult)
                b.stt(yo[par][:, 0:n], o[:, 0:n], VT[:, 72:73], gt[par][:, 0:n], ALU.mult, ALU.mult)
                b.ld(dr["YHG"][rows_, t0:t0 + n], yo[par][:, 0:n], q="pool")
        p.barrier()
    eso.close()


def phase_f(st, l, last, src, dst):
    b = st["b"]; nc, p, dr = b.nc, b.p, b.dr
    ident, ones32, VT, epsc = st["ident"], st["ones32"], st["VT"], st["epsc"]
    with ExitStack() as es:
        sb = lambda n, s, dt=F32: st["sb"](n + f"_f{l}", s, dt, es)
        ps = lambda n, s, dt=F32: st["ps"](n + f"_f{l}", s, dt, es)
        dwr = sb("dwr", (31, 1024))
        b.ld(dwr[:], dr["conv_dw"][l])
        dwT = sb("dwT", (128, 8, 31))
        pT = ps("pT", (128, 512))
        for c in range(8):
            b.tr(pT[:, c * 32:c * 32 + 31], dwr[:, c * 128:(c + 1) * 128], ident[0:31, 0:31])
        for c in range(8):
            b.cp("dve", dwT[:, c, :], pT[:, c * 32:c * 32 + 31])
        DG = sb("DG", (128, 248, 128), BF16)
        for c in range(8):
            for k in range(31):
                b.ts("dve" if (c * 31 + k) % 2 == 0 else "pool", K(DG[:, c * 31 + k, :], f"DG{c}"), ident[:], dwT[:, c, k:k + 1], None, ALU.mult)
        ub = [sb(f"ub{i}", (128, 8, 542), BF16) for i in range(2)]
        hal = [sb(f"hal{i}", (128, 8, 15), BF16) for i in range(2)]
        CV = sb("CV", (128, 8, 512)); SQ = sb("SQ", (128, 8, 512))
        ycs = [sb(f"ycs{i}", (128, 8, 512), BF16) for i in range(2)]
        mean = sb("mean", (128, 512)); msq = sb("msq", (128, 512)); rstd = sb("rstd", (128, 512))
        pc = [ps(f"pc{i}", (128, 512)) for i in range(4)]
        pss = ps("pss", (128, 512)); psq = ps("psq", (128, 512))
        npc = 0
        for gi, (t0, n) in enumerate(GROUPS):
            if last and t0 < NCTX:
                continue
            s0, s1 = (0, NCTX) if t0 < NCTX else (NCTX, NT)
            lo, hi = t0 - 15, t0 + n + 15
            clo, chi = max(lo, s0), min(hi, s1)
            u = ub[gi % 2]
            if clo != lo or chi != hi:
                b.memset("pool", u[:], 0.0)
            b.ld(u[:, :, clo - lo:chi - lo], dr["UT"][:, clo:chi].rearrange("(c p) t -> p c t", p=128))
            if t0 == NCTX:
                b.ld(hal[0][:], dr["UEF"][0:1024, 16:31].rearrange("(c p) t -> p c t", p=128))
                b.ts("dve", u[:, :, 0:15], hal[0][:], st["rolem"][:, 0:1], None, ALU.mult)
            if t0 + n == NT:
                b.ld(hal[1][:], dr["UEF"][1024:2048, 0:15].rearrange("(c p) t -> p c t", p=128))
                b.ts("dve", u[:, :, n + 15:n + 30], hal[1][:], st["rolem"][:, 1:2], None, ALU.mult)
            for c in range(8):
                pcc = pc[npc % 4]; npc += 1
                for k in range(31):
                    b.mm(pcc[:, 0:n], K(DG[:, c * 31 + k, :], f"DG{c}"), u[:, c, k:k + n], start=(k == 0), stop=(k == 30))
                b.act(K(CV[:, c, 0:n], f"CV{c}"), pcc[:, 0:n], AF.Identity, bias=VT[:, 48 + c:49 + c])
                b.act(K(SQ[:, c, 0:n], f"SQ{c}"), pcc[:, 0:n], AF.Square, bias=VT[:, 48 + c:49 + c])
            for c in range(8):
                b.mm(pss[:, 0:n], ones32[:], K(CV[:, c, 0:n], f"CV{c}"), start=(c == 0), stop=(c == 7))
            for c in range(8):
                b.mm(psq[:, 0:n], ones32[:], K(SQ[:, c, 0:n], f"SQ{c}"), start=(c == 0), stop=(c == 7))
            b.ts("dve", mean[:, 0:n], pss[:, 0:n], 1.0 / 1024.0, None, ALU.mult)
            b.tt("dve", msq[:, 0:n], mean[:, 0:n], mean[:, 0:n], ALU.mult)
            b.stt(rstd[:, 0:n], psq[:, 0:n], 1.0 / 1024.0, msq[:, 0:n], ALU.mult, ALU.subtract)
            b.act(rstd[:, 0:n], rstd[:, 0:n], AF.Sqrt, bias=epsc[:, 0:1])
            b.recip(rstd[:, 0:n], rstd[:, 0:n])
            y = ycs[gi % 2]
            for c in range(8):
                cv = K(CV[:, c, 0:n], f"CV{c}")
                b.tt("dve", cv, cv, mean[:, 0:n], ALU.subtract)
                b.tt("pool", cv, cv, rstd[:, 0:n], ALU.mult)
                b.act(y[:, c, 0:n], cv, AF.Silu, bias=VT[:, 64 + c:65 + c], scale=VT[:, 56 + c:57 + c])
            b.ld(dr["YCONV"][:, t0:t0 + n].rearrange("(c p) t -> p c t", p=128), y[:, :, 0:n], q="pool")
        p.barrier()


def phase_g(st, l, last, src, dst):
    b = st["b"]; nc, p, dr = b.nc, b.p, b.dr
    onesb = st["onesb"]
    with ExitStack() as es:
        sb = lambda n, s, dt=F32: st["sb"](n + f"_g{l}", s, dt, es)
        ps = lambda n, s, dt=F32: st["ps"](n + f"_g{l}", s, dt, es)
        NK = NCTX + NXF
        KTr = sb("KTr", (128, 4, NK), BF16)
        Vr = sb("Vr", (128, NKT, 512), BF16)
        for h in range(4):
            b.ld(K(KTr[:, h, 0:NCTX], "KTr"), dr["KTA"][h * 128:(h + 1) * 128, :])
            for r_ in range(2):
                b.ld(K(KTr[:, h, NCTX + r_ * NX:NCTX + (r_ + 1) * NX], "KTr"), dr["KXF"][r_ * 512 + h * 128:r_ * 512 + (h + 1) * 128, :])
        b.ld(K(Vr[:, 0:2, :], "Vr"), dr["VATT"].rearrange("(t p) f -> p t f", p=128))
        b.ld(K(Vr[:, 2:NKT, :], "Vr"), dr["VXF"].rearrange("(t p) f -> p t f", p=128))
        qg = [sb(f"qg{i}", (128, 8, 512), BF16) for i in range(2)]
        yat = [sb(f"yat{i}", (128, 8, 512), BF16) for i in range(2)]
        pex = [sb(f"pex{i}", (128, 512), BF16) for i in range(3)]
        rin = [sb(f"rin{i}", (128, 512)) for i in range(2)]
        psc = [ps(f"psc{i}", (128, 512)) for i in range(3)]
        pO = [ps(f"pO{i}", (128, 512)) for i in range(2)]
        pR = [ps(f"pR{i}", (128, 512)) for i in range(2)]
        scale = 128.0 ** -0.5
        items = []
        for gi, (t0, n) in enumerate(GROUPS):
            if t0 < NCTX:
                if last:
                    continue
                ktiles = [0, 1]
            else:
                ktiles = list(range(NKT))
            for h in range(8):
                for ki, kt in enumerate(ktiles):
                    items.append((gi, t0, n, h, kt, ki == 0, ki == len(ktiles) - 1))
        loaded = set()
        hidx = {}

        def emit_sc(i):
            gi, t0, n, h, kt, first, lastk = items[i]
            q = qg[gi % 2]
            if gi not in loaded:
                loaded.add(gi)
                b.ld(q[:, :, 0:n], dr["QTA"][:, t0:t0 + n].rearrange("(h p) t -> p h t", p=128))
            b.mm(psc[i % 3][:, 0:n], K(KTr[:, h // 2, kt * 128:(kt + 1) * 128], "KTr"), q[:, h, 0:n])

        for i in range(min(2, len(items))):
            emit_sc(i)
        for i, (gi, t0, n, h, kt, first, lastk) in enumerate(items):
            kv = h // 2
            if (gi, h) not in hidx:
                hidx[(gi, h)] = len(hidx)
            nh = hidx[(gi, h)]
            po = pO[nh % 2]; pr = pR[nh % 2]; ri = rin[nh % 2]
            y = yat[gi % 2]
            pe_ = pex[i % 3]
            b.act(pe_[:, 0:n], psc[i % 3][:, 0:n], AF.Exp, scale=scale)
            if i + 2 < len(items):
                emit_sc(i + 2)
            b.mm(po[:, 0:n], K(Vr[:, kt, kv * 128:(kv + 1) * 128], "Vr"), pe_[:, 0:n], start=first, stop=lastk)
            b.mm(pr[:, 0:n], onesb[:], pe_[:, 0:n], start=first, stop=lastk)
            if lastk:
                b.recip(ri[:, 0:n], pr[:, 0:n])
                b.tt("dve", y[:, h, 0:n], po[:, 0:n], ri[:, 0:n], ALU.mult)
                if h == 7:
                    b.ld(dr["YATT"][:, t0:t0 + n].rearrange("(h p) t -> p h t", p=128), y[:, :, 0:n], q="pool")
        p.barrier()


def ln_tile(b, r, xo, gbc, bbc, st6, mv, epsc):
    nc, p = b.nc, b.p
    p.op("dve", lambda: nc.vector.bn_stats(st6[:, 0, :], r[:, 0:512]), [r[:]], [st6[:]])
    p.op("dve", lambda: nc.vector.bn_stats(st6[:, 1, :], r[:, 512:1024]), [r[:]], [st6[:]])
    p.op("dve", lambda: nc.vector.bn_aggr(mv[:, 0:2], st6[:].rearrange("p a b -> p (a b)")), [st6[:]], [mv[:]])
    b.act(mv[:, 2:3], mv[:, 1:2], AF.Sqrt, bias=epsc[:, 0:1])
    b.recip(mv[:, 3:4], mv[:, 2:3])
    b.ts("dve", r[:], r[:], mv[:, 0:1], mv[:, 3:4], ALU.subtract, ALU.mult)
    b.tt("pool", r[:], r[:], gbc[:], ALU.mult)
    b.tt("pool", xo[:], r[:], bbc[:], ALU.add)


def phase_h(st, l, last, src, dst):
    b = st["b"]; nc, p, dr = b.nc, b.p, b.dr
    MODBC, epsc = st["MODBC"], st["epsc"]
    with ExitStack() as es:
        sb = lambda n, s, dt=F32: st["sb"](n + f"_h{l}", s, dt, es)
        ps = lambda n, s, dt=F32: st["ps"](n + f"_h{l}", s, dt, es)
        wst = sb("wst", (128, 8, 256))
        W = []
        for wi, wn in enumerate(("w_hg_o", "w_conv_o", "w_att_o", "w_out")):
            w = sb(f"W{wi}", (128, 8, 1024), BF16)
            for q4 in range(4):
                b.ld(wst[:], dr[wn][l, :, q4 * 256:(q4 + 1) * 256].rearrange("(k p) c -> p k c", p=128))
                b.cp("pool" if q4 % 2 else "dve", w[:, :, q4 * 256:(q4 + 1) * 256], wst[:])
            W.append(w)
        lng = sb("lng", (128, 1024)); lnb = sb("lnb", (128, 1024))
        b.ld(lng[:], dr["ln1_g"][l].partition_broadcast(128), q="pool")
        b.ld(lnb[:], dr["ln1_b"][l].partition_broadcast(128), q="pool")
        Y = [sb(f"Y{i}", (128, 8, 512), BF16) for i in range(3)]
        MGs = sb("MGs", (128, 24, 512), BF16)
        mrg = sb("mrg", (128, 8, 512), BF16)
        tm = [sb(f"tm{i}", (128, 512)) for i in range(3)]
        xt = [sb(f"xt{i}", (128, 1024)) for i in range(2)]
        rr = [sb(f"rr{i}", (128, 1024)) for i in range(2)]
        st6 = sb("st6", (128, 2, 6)); mv = sb("mv", (128, 4))
        pb = [ps(f"pb{i}", (128, 512)) for i in range(6)]
        pm = ps("pm", (128, 1024))
        nj = 0; ntl = 0
        for (t0, n) in GROUPS:
            if last and t0 < NCTX:
                continue
            v = 1 if t0 < NCTX else 0
            for i, nm in enumerate(("YHG", "YCONV", "YATT")):
                b.ld(Y[i][:, :, 0:n], dr[nm][:, t0:t0 + n].rearrange("(k p) t -> p k t", p=128))
            b.ld(MGs[:, :, 0:n], dr["MG"][:, t0:t0 + n].rearrange("(c p) t -> p c t", p=128))
            for c in range(8):
                pbs = pb[3 * (nj % 2):3 * (nj % 2) + 3]; nj += 1
                for i in range(3):
                    for k in range(8):
                        b.mm(pbs[i][:, 0:n], W[i][:, k, c * 128:(c + 1) * 128], Y[i][:, k, 0:n], start=(k == 0), stop=(k == 7))
                for i in range(3):
                    b.tt("dve", tm[i][:, 0:n], pbs[i][:, 0:n], MGs[:, i * 8 + c, 0:n], ALU.mult)
                b.tt("pool", tm[0][:, 0:n], tm[0][:, 0:n], tm[1][:, 0:n], ALU.add)
                b.tt("pool", K(mrg[:, c, 0:n], f"mrg{c}"), tm[0][:, 0:n], tm[2][:, 0:n], ALU.add)
            for tt_ in range(n // 128):
                par = ntl % 2; ntl += 1
                r0 = t0 + tt_ * 128
                x_ = xt[par]; r_ = rr[par]
                b.ld(x_[:], src[r0:r0 + 128, :])
                for nn in range(2):
                    for k in range(8):
                        b.mm(K(pm[:, nn * 512:(nn + 1) * 512], f"pm{nn}"), K(mrg[:, k, tt_ * 128:(tt_ + 1) * 128], f"mrg{k}"),
                             W[3][:, k, nn * 512:(nn + 1) * 512], start=(k == 0), stop=(k == 7))
                for nn in range(2):
                    b.tt("dve", r_[:, nn * 512:(nn + 1) * 512], K(pm[:, nn * 512:(nn + 1) * 512], f"pm{nn}"),
                         MODBC[:, v, 0, nn * 512:(nn + 1) * 512], ALU.mult)
                b.stt(r_[:], x_[:], ALPHA, r_[:], ALU.mult, ALU.add)
                ln_tile(b, r_, r_, lng, lnb, st6, mv, epsc)
                b.ld(dr["X1"][r0:r0 + 128, :], r_[:], q="pool")
        p.barrier()


def phase_tab(st):
    b = st["b"]; nc, p, dr = b.nc, b.p, b.dr
    with ExitStack() as es:
        sb = lambda n, s, dt=F32: st["sb"](n + "_tab", s, dt, es)
        tin = [sb(f"tin{i}", (128, 4, 1024)) for i in range(3)]
        tout = [sb(f"tout{i}", (128, 4, 1024), BF16) for i in range(3)]
        it = 0
        for tbl, col in (("peer_u", 0), ("peer_v", 1024)):
            srcT = dr[tbl].rearrange("l e d -> (l e) d")
            for blk in range(2 * 16384 // 512):
                r0 = blk * 512
                ti_, to_ = tin[it % 3], tout[it % 3]
                b.ld(ti_[:], srcT[r0:r0 + 512, :].rearrange("(p r) d -> p r d", r=4))
                b.cp("dve" if it % 2 == 0 else "act", to_[:], ti_[:])
                b.ld(dr["UV16"][r0:r0 + 512, col:col + 1024].rearrange("(p r) d -> p r d", r=4), to_[:], q="pool")
                it += 1
        p.barrier()


NGB = 12
GEL = 4


def phase_i(st, l, last, src, dst):
    b = st["b"]; nc, p, dr = b.nc, b.p, b.dr
    ident, MODBC, epsc = st["ident"], st["MODBC"], st["epsc"]
    with ExitStack() as es:
        sb = lambda n, s, dt=F32: st["sb"](n + f"_i{l}", s, dt, es)
        ps = lambda n, s, dt=F32: st["ps"](n + f"_i{l}", s, dt, es)
        wqs = [sb(f"wqs{i}", (128, 8, 512)) for i in range(2)]
        kraw = sb("kraw", (128, 2, 128)); kT = sb("kT", (128, 2, 128))
        b.ld(kraw[:, 0, :], dr["peer_k1"][l]); b.ld(kraw[:, 1, :], dr["peer_k2"][l])
        pT = [ps(f"pT{i}", (128, 512)) for i in range(2)]
        for i in range(2):
            b.tr(pT[0][:, i * 128:(i + 1) * 128], kraw[:, i, :], ident[:])
        b.cp("dve", kT[:], pT[0][:, 0:256].rearrange("p (a c) -> p a c", c=128))
        lng = sb("lng", (128, 1024)); lnb = sb("lnb", (128, 1024)); io = sb("io", (128, 16))
        b.ld(lng[:], dr["ln2_g"][l].partition_broadcast(128), q="pool")
        b.ld(lnb[:], dr["ln2_b"][l].partition_broadcast(128), q="pool")
        b.ld(io[:], dr["iota256"][:, 0:16])
        X1 = [sb(f"x1{i}", (128, 1024)) for i in range(2)]
        HH = [sb(f"hh{i}", (128, 1024)) for i in range(2)]
        HH16 = [sb(f"hh16{i}", (128, 1024), BF16) for i in range(2)]
        EIDs = [sb(f"EID{i}", (128, 128), I32) for i in range(2)]
        GTS = [sb(f"GTs{i}", (128, 8, 16)) for i in range(2)]
        hT = sb("hT", (128, 8, 128))
        qT = [sb(f"qT{i}", (128, 4, 128)) for i in range(2)]
        SC = sb("SC", (128, 16, 128)); SC2 = sb("SC2", (128, 16, 128))
        oh = sb("oh", (128, 8, 16, 16))
        V1 = sb("V1", (128, 16, 16)); I1u = sb("I1u", (128, 16, 16), U32); I1f = sb("I1f", (128, 16, 16))
        SCO = sb("SCO", (128, 8, 16)); CIu = sb("CIu", (128, 8, 16), U32)
        hiU = sb("hiU", (128, 8, 16), U32); loU = sb("loU", (128, 8, 16), U32)
        hiF = sb("hiF", (128, 8, 16)); loF = sb("loF", (128, 8, 16))
        s1 = sb("s1", (128, 8, 16)); s2 = sb("s2", (128, 8, 16))
        SEL = sb("SEL", (128, 128))
        Z = sb("Z", (128, 8)); ACTV = sb("ACTV", (128, 128)); t1 = sb("t1", (128, 128))
        Wt = sb("Wt", (128, 128))
        gb = [sb(f"gb{i}", (128, 2048), BF16) for i in range(NGB)]
        junk = sb("junk", (128, 1024), BF16); acc = sb("acc", (128, 1024))
        st6 = sb("st6", (128, 2, 6)); mv = sb("mv", (128, 4))
        pq = [ps(f"pq{i}", (128, 512)) for i in range(2)]
        psS = [ps(f"psS{i}", (128, 512)) for i in range(2)]
        pv = ps("pv", (128, 1024))
        identr = sb("identr", (128, 128), F32R)
        b.cp("dve", identr[:], ident[:])
        tmpv = [sb(f"tmpv{i}", (128, 1024), F32R) for i in range(3)]

        def route(ti, par):
            v = 1 if ti < 2 else 0
            r0 = ti * 128
            x1, hh, EID, GT_ = X1[par], HH[par], EIDs[par], GTS[par]
            b.ld(x1[:], dr["X1"][r0:r0 + 128, :]); yield
            b.tt("dve", hh[:], x1[:], MODBC[:, v, 3, :], ALU.mult); yield
            b.tt("pool", hh[:], hh[:], MODBC[:, v, 2, :], ALU.add); yield
            b.cp("pool", HH16[par][:], hh[:]); yield
            for hf in range(2):
                for j in range(4):
                    k = hf * 4 + j
                    b.tr(pT[hf][:, j * 128:(j + 1) * 128], hh[:, k * 128:(k + 1) * 128], ident[:])
                b.cp("act", hT[:, hf * 4:(hf + 1) * 4, :], pT[hf][:].rearrange("p (a c) -> p a c", c=128)); yield
            for g4 in range(4):
                pq_ = pq[g4 % 2]; qt = qT[g4 % 2]; pss = psS[g4 % 2]
                wq_ = wqs[g4 % 2]
                b.ld(wq_[:], dr["peer_wq"][l, :, g4 * 512:(g4 + 1) * 512].rearrange("(k p) c -> p k c", p=128)); yield
                for j in range(4):
                    cc = g4 * 4 + j
                    for k in range(8):
                        b.mm(pq_[:, j * 128:(j + 1) * 128], wq_[:, k, j * 128:(j + 1) * 128], hT[:, k, :],
                             start=(k == 0), stop=(k == 7))
                b.cp("act", qt[:], pq_[:].rearrange("p (a c) -> p a c", c=128)); yield
                for j in range(4):
                    cc = g4 * 4 + j
                    b.mm(pss[:, j * 128:(j + 1) * 128], qt[:, j, :], kT[:, cc % 2, :])
                b.cp("act", SC[:, g4 * 4:(g4 + 1) * 4, :], pss[:].rearrange("p (a c) -> p a c", c=128)); yield
            kV = lambda cc: f"V1_{cc}"
            for cc in range(16):
                p.op("dve", lambda: nc.vector.max(V1[:, cc, 0:8], SC[:, cc, :]), [SC[:]], [kV(cc)]); yield
            for cc in range(16):
                p.op("dve", lambda: nc.vector.max_index(I1u[:, cc, 0:8], V1[:, cc, 0:8], SC[:, cc, :]), [SC[:], kV(cc)], [f"I1_{cc}"]); yield
            for cc in range(16):
                p.op("dve", lambda: nc.vector.match_replace(SC2[:, cc, :], V1[:, cc, 0:8], SC[:, cc, :], -1e30), [SC[:], kV(cc)], [f"SC2_{cc}"]); yield
            for cc in range(16):
                p.op("dve", lambda: nc.vector.max(V1[:, cc, 8:16], SC2[:, cc, :]), [f"SC2_{cc}"], [f"V1b_{cc}"]); yield
            for cc in range(16):
                p.op("dve", lambda: nc.vector.max_index(I1u[:, cc, 8:16], V1[:, cc, 8:16], SC2[:, cc, :]), [f"SC2_{cc}", f"V1b_{cc}"], [I1u[:], V1[:], SC2[:]]); yield
            b.cp("dve", I1f[:], I1u[:]); yield
            cnd = SC[:].rearrange("p a c -> p (a c)").rearrange("p (h i j) -> p h i j", h=8, i=16)
            for h in range(8):
                b.tt("dve", cnd[:, h], V1[:, 2 * h, :, None].to_broadcast([128, 16, 16]),
                     V1[:, 2 * h + 1, None, :].to_broadcast([128, 16, 16]), ALU.add); yield
            cflat = SC[:].rearrange("p a c -> p (a c)").rearrange("p (h c) -> p h c", h=8)
            C2 = SC2[:].rearrange("p a c -> p (a c)").rearrange("p (h c) -> p h c", h=8)
            for h in range(8):
                p.op("dve", lambda: nc.vector.max(SCO[:, h, 0:8], cflat[:, h, :]), [SC[:]], [f"SCO_{h}"]); yield
            for h in range(8):
                p.op("dve", lambda: nc.vector.max_index(CIu[:, h, 0:8], SCO[:, h, 0:8], cflat[:, h, :]), [SC[:], f"SCO_{h}"], [f"CI_{h}"]); yield
            for h in range(8):
                p.op("dve", lambda: nc.vector.match_replace(C2[:, h, :], SCO[:, h, 0:8], cflat[:, h, :], -1e30), [SC[:], f"SCO_{h}"], [f"C2_{h}"]); yield
            for h in range(8):
                p.op("dve", lambda: nc.vector.max(SCO[:, h, 8:16], C2[:, h, :]), [f"C2_{h}"], [f"SCOb_{h}"]); yield
            for h in range(8):
                p.op("dve", lambda: nc.vector.max_index(CIu[:, h, 8:16], SCO[:, h, 8:16], C2[:, h, :]), [f"C2_{h}", f"SCOb_{h}"], [CIu[:], SCO[:], SC2[:]]); yield
            p.op("dve", lambda: nc.vector.tensor_single_scalar(hiU[:], CIu[:], 4, ALU.logical_shift_right), [CIu[:]], [hiU[:]]); yield
            p.op("dve", lambda: nc.vector.tensor_single_scalar(loU[:], CIu[:], 15, ALU.bitwise_and), [CIu[:]], [loU[:]]); yield
            b.cp("dve", hiF[:], hiU[:]); yield
            b.cp("dve", loF[:], loU[:]); yield
            I1v = I1f[:].rearrange("p (h a) i -> p h a i", a=2)
            iob = io[:, None, None, :].to_broadcast([128, 8, 16, 16])
            for (xf, half, so) in ((hiF, 0, s1), (loF, 1, s2)):
                b.tt("dve", oh[:], iob, xf[:, :, :, None].to_broadcast([128, 8, 16, 16]), ALU.is_equal); yield
                b.tt("dve", oh[:], oh[:], I1v[:, :, half, None, :].to_broadcast([128, 8, 16, 16]), ALU.mult); yield
                p.op("dve", lambda: nc.vector.tensor_reduce(so[:], oh[:], AX.X, ALU.add), [oh[:]], [so[:]]); yield
            b.stt(SEL[:], s1[:].rearrange("p a b -> p (a b)"), 128.0, s2[:].rearrange("p a b -> p (a b)"), ALU.mult, ALU.add); yield
            if l > 0:
                b.ts("dve", SEL[:], SEL[:], float(l * 16384), None, ALU.add); yield
            b.cp("dve", EID[:], SEL[:]); yield
            b.tt("dve", GT_[:], SCO[:], SCO[:, :, 0:1].to_broadcast([128, 8, 16]), ALU.subtract); yield
            b.act(GT_[:], GT_[:], AF.Exp); yield
            p.op("dve", lambda: nc.vector.tensor_reduce(Z[:], GT_[:], AX.X, ALU.add), [GT_[:]], [Z[:]]); yield
            b.recip(Z[:], Z[:]); yield
            b.tt("dve", GT_[:], GT_[:], Z[:, :, None].to_broadcast([128, 8, 16]), ALU.mult); yield

        ngc = [0]

        def experts(ti, par, nxt):
            v = 1 if ti < 2 else 0
            r0 = ti * 128
            x1, hh, EID, GT_ = X1[par], HH[par], EIDs[par], GTS[par]
            step = (lambda: next(nxt, None)) if nxt is not None else (lambda: None)
            hh16 = HH16[par]
            gtf = GT_[:].rearrange("p h k -> p (h k)")
            for s0 in range(0, 128, GEL):
                slots = []
                for s in range(s0, s0 + GEL):
                    g_ = gb[ngc[0] % NGB]; ngc[0] += 1
                    slots.append(g_)
                    p.gather(g_[:], dr["UV16"], EID[:, s:s + 1])
                    b.stt(junk[:], g_[:, 0:1024], 1.0, hh16[:], ALU.mult, ALU.mult, accum_out=ACTV[:, s:s + 1])
                    step()
                cs = slice(s0, s0 + GEL)
                b.act(t1[:, cs], ACTV[:, cs], AF.Gelu_apprx_tanh)
                b.tt("dve", Wt[:, cs], t1[:, cs], gtf[:, cs], ALU.mult)
                for s, g_ in zip(range(s0, s0 + GEL), slots):
                    tv_ = tmpv[s % 3]
                    b.act(tv_[:], g_[:, 1024:2048], AF.Copy, scale=Wt[:, s:s + 1])
                    for nn in range(2):
                        b.mm(K(pv[:, nn * 512:(nn + 1) * 512], f"pv{nn}"), identr[:], tv_[:, nn * 512:(nn + 1) * 512],
                             start=(s == 0), stop=(s == 127))
            for nn in range(2):
                b.tt("dve", acc[:, nn * 512:(nn + 1) * 512], K(pv[:, nn * 512:(nn + 1) * 512], f"pv{nn}"),
                     MODBC[:, v, 1, nn * 512:(nn + 1) * 512], ALU.mult)
            b.stt(acc[:], x1[:], ALPHA, acc[:], ALU.mult, ALU.add)
            ln_tile(b, acc, acc, lng, lnb, st6, mv, epsc)
            if last:
                b.ld(dst[r0 - NCTX:r0 - NCTX + 128, :], acc[:], q="sp")
            else:
                b.ld(dst[r0:r0 + 128, :], acc[:], q="sp")

        tiles = [ti for ti in range(NTILE) if not (last and ti < 2)]
        g0 = route(tiles[0], 0)
        for _ in g0:
            pass
        for idx, ti in enumerate(tiles):
            nxt = route(tiles[idx + 1], (idx + 1) % 2) if idx + 1 < len(tiles) else None
            experts(ti, idx % 2, nxt)
            if nxt is not None:
                for _ in nxt:
                    pass
        p.barrier()


_CACHE = {}


def kernel(**inputs):
    if "bld" not in _CACHE:
        _CACHE["bld"] = build()
    bld = _CACHE["bld"]
    consts = host_consts()
    rope_full = consts.pop("ropecs_full")
    f32 = lambda a: np.ascontiguousarray(np.asarray(a, dtype=np.float32))
    shared = {n: f32(inputs[n]) for n, _ in WNAMES}
    shared.update(consts)
    x, c, ctx, c_ctx = (f32(inputs[k]) for k in ("x", "c", "ctx", "c_ctx"))
    nb = x.shape[0]
    in_maps = []
    for core in range(8):
        bi, role = (core // 2) % nb, core % 2
        m = dict(shared)
        m["xin"] = np.ascontiguousarray(np.concatenate([ctx[bi], x[bi, role * NX:(role + 1) * NX]], axis=0))
        m["cvec"] = np.ascontiguousarray(np.concatenate([c[bi].reshape(8, 128), c_ctx.reshape(8, 128)], axis=0))
        m["ropecs"] = np.ascontiguousarray(rope_full[role * NX:(role + 1) * NX])
        rm = np.zeros((128, 2), np.float32)
        rm[:, 0] = float(role == 1)
        rm[:, 1] = float(role == 0)
        m["rolem"] = rm
        in_maps.append(m)
    res = run_bass_kernel_spmd(bld.nc, in_maps, core_ids=list(range(8)))
    out = np.empty((nb, NXF, D), np.float32)
    for core in range(2 * nb):
        bi, role = core // 2, core % 2
        out[bi, role * NX:(role + 1) * NX] = np.asarray(res.results[core]["y"], dtype=np.float32)
    return out
```

```python
import numpy as np
from contextlib import ExitStack
import concourse.bass as bass
import concourse.mybir as mybir
from concourse.bass_utils import run_bass_kernel_spmd

F32 = mybir.dt.float32
F32R = mybir.dt.float32r
BF16 = mybir.dt.bfloat16
I32 = mybir.dt.int32
U32 = mybir.dt.uint32
AF = mybir.ActivationFunctionType
ALU = mybir.AluOpType
AX = mybir.AxisListType

D = 1024
NCTX = 256
NXF = 4096
NX = 2048
NT = NCTX + NX
NTILE = NT // 128
NKT = (NCTX + NXF) // 128
DEPTH = 2
INW = 12288
EPS = 1e-6
ALPHA = (2 * DEPTH) ** 0.25
CH = 16
NCH = NT // CH
C_HGQ, C_HGFF, C_HGFB, C_HGI, C_HGG, C_GLUA, C_GLUG, C_ATQ, C_ATK, C_ATV, C_MG = (
    0, 1024, 2048, 3072, 4096, 5120, 6144, 7168, 8192, 8704, 9216)

NDMASEM = 24


class Prog:
    def __init__(self, nc):
        self.nc = nc
        self.eng = {"pe": nc.tensor, "dve": nc.vector, "act": nc.scalar,
                    "pool": nc.gpsimd, "sp": nc.sync}
        self.sem = {k: nc.alloc_semaphore(name="es_" + k) for k in self.eng}
        self.cnt = {k: 0 for k in self.eng}
        self.dsem = {q: [nc.alloc_semaphore(name=f"ds_{q}_{i}") for i in range(NDMASEM)]
                     for q in ("sp", "pool", "act")}
        self.dcnt = {q: 0 for q in self.dsem}
        self.dval = {q: [0] * NDMASEM for q in self.dsem}
        self.known = {k: {} for k in self.eng}
        self.res = {}
        self.ninstr = 0
        self.rr = 0
        self.ccsems = []
        self.ccs = []

    def _r(self, key):
        r = self.res.get(key)
        if r is None:
            r = self.res[key] = [{}, {}]
        return r

    def _need(self, e, tok):
        sh, sname, val, src = tok
        if src == e and e == "pe":
            return
        if self.known[e].get(sname, 0) >= val:
            return
        self.eng[e].wait_ge(sh, val)
        self.known[e][sname] = val
        self.ninstr += 1

    def _deps(self, e, rkeys, wkeys, is_dma=False):
        for k in rkeys:
            for tok in self._r(k)[0].values():
                self._need(e, tok)
        for k in wkeys:
            r = self._r(k)
            for tok in r[0].values():
                w_is_dma = tok[3].startswith("dma") or tok[3].startswith("cc")
                if is_dma and w_is_dma:
                    continue
                if is_dma or w_is_dma or tok[3] != e:
                    self._need(e, tok)
            for tok in r[1].values():
                if is_dma or tok[3] != e:
                    self._need(e, tok)

    def _mark(self, tok, rkeys, wkeys):
        sname = tok[1]
        is_dma = tok[3].startswith("dma") or tok[3].startswith("cc")
        for k in rkeys:
            self._r(k)[1][sname] = tok
        for k in wkeys:
            r = self._r(k)
            if is_dma:
                r[0] = {n: t for n, t in r[0].items() if t[3].startswith("dma") or t[3].startswith("cc")}
                r[0][sname] = tok
            else:
                r[0] = {sname: tok}
            r[1] = {}

    @staticmethod
    def _split(lst):
        aps, keys = [], []
        for x in lst:
            if isinstance(x, tuple):
                aps.append(x[0]); keys.append(x[1])
            elif isinstance(x, str):
                keys.append(x)
            else:
                aps.append(x); keys.append(x.name)
        return aps, keys

    def op(self, e, fn, ins, outs):
        _, rk = self._split(ins)
        _, wk = self._split(outs)
        self._deps(e, rk, wk)
        ins_ = fn()
        self.cnt[e] += 1
        ins_.then_inc(self.sem[e], 1)
        tok = (self.sem[e], "es_" + e, self.cnt[e], e)
        self._mark(tok, rk, wk)
        self.ninstr += 1
        return ins_

    def dma(self, q, out, in_, extra_in=(), **kw):
        oa, wk = self._split([out])
        ia, rk = self._split([in_] + list(extra_in))
        self._deps(q, rk, wk, is_dma=True)
        i = self.dcnt[q] % NDMASEM
        self.dcnt[q] += 1
        self.dval[q][i] += 16
        sh = self.dsem[q][i]
        ins_ = self.eng[q].dma_start(out=oa[0], in_=ia[0], **kw)
        ins_.then_inc(sh, 16)
        tok = (sh, f"ds_{q}_{i}", self.dval[q][i], f"dma_{q}_{self.dcnt[q]}")
        self._mark(tok, rk, wk)
        self.ninstr += 1
        return tok

    def gather(self, out, table, idx_ap, extra_in=()):
        q = "pool"
        oa, wk = self._split([out])
        ia, rk = self._split([table, idx_ap] + list(extra_in))
        self._deps(q, rk, wk, is_dma=True)
        i = self.dcnt[q] % NDMASEM
        self.dcnt[q] += 1
        self.dval[q][i] += 16
        sh = self.dsem[q][i]
        ins_ = self.nc.gpsimd.indirect_dma_start(
            out=oa[0], out_offset=None, in_=ia[0],
            in_offset=bass.IndirectOffsetOnAxis(ap=ia[1], axis=0))
        ins_.then_inc(sh, 16)
        tok = (sh, f"ds_{q}_{i}", self.dval[q][i], f"dma_{q}_{self.dcnt[q]}")
        self._mark(tok, rk, wk)
        self.ninstr += 1
        return tok

    def allgather_pairs(self, in_ap, out_ap):
        q = "pool"
        ia, rk = self._split([in_ap]); oa, wk = self._split([out_ap])
        self._deps(q, rk, wk, is_dma=True)
        sh = self.nc.alloc_semaphore(name=f"cc_{len(self.ccsems)}")
        self.ccsems.append(sh)
        ins_ = self.nc.gpsimd.collective_compute(
            "AllGather", ALU.bypass, replica_groups=[[0, 1], [2, 3], [4, 5], [6, 7]],
            ins=[ia[0].opt()], outs=[oa[0].opt()])
        ins_.then_inc(sh)
        tok = (sh, f"cc_{len(self.ccsems)}", 1, f"cc_{len(self.ccsems)}")
        self._mark(tok, rk, wk)
        self.ccs.append(tok)
        self.ninstr += 1
        return tok

    def ldq(self):
        self.rr += 1
        return "sp"

    def barrier(self):
        e = "sp"
        for k in ("pe", "dve", "act", "pool"):
            if self.cnt[k]:
                self._need(e, (self.sem[k], "es_" + k, self.cnt[k], k))
        for q in self.dsem:
            for i in range(NDMASEM):
                if self.dval[q][i]:
                    self._need(e, (self.dsem[q][i], f"ds_{q}_{i}", self.dval[q][i], "dma"))
        for tok in self.ccs:
            self._need(e, tok)
        ins_ = self.nc.sync.nop()
        self.cnt[e] += 1
        ins_.then_inc(self.sem[e], 1)
        tok = (self.sem[e], "es_sp", self.cnt[e], "sp")
        for k in ("pe", "dve", "act", "pool"):
            self._need(k, tok)
        self.res = {}


class B:
    def __init__(self, debug=()):
        self.nc = nc = bass.Bass("TRN2", target_bir_lowering=False)
        self.p = Prog(nc)
        self.debug = set(debug)
        self.dr = {}

    def inp(self, name, shape, dt=F32):
        self.dr[name] = self.nc.dram_tensor(name, list(shape), dt, kind="ExternalInput").ap()
        return self.dr[name]

    def outp(self, name, shape, dt=F32):
        self.dr[name] = self.nc.dram_tensor(name, list(shape), dt, kind="ExternalOutput").ap()
        return self.dr[name]

    def scr(self, name, shape, dt=F32):
        kind = "ExternalOutput" if name in self.debug else "Internal"
        self.dr[name] = self.nc.dram_tensor(name, list(shape), dt, kind=kind).ap()
        return self.dr[name]

    def mm(self, out, lhsT, rhs, start=True, stop=True):
        nc = self.nc
        return self.p.op("pe", lambda: nc.tensor.matmul(_a(out), _a(lhsT), _a(rhs), start=start, stop=stop),
                         [lhsT, rhs], [out])

    def tr(self, out, in_, ident):
        nc = self.nc
        return self.p.op("pe", lambda: nc.tensor.transpose(_a(out), _a(in_), _a(ident)), [in_, ident], [out])

    def act(self, out, in_, func, bias=None, scale=None, e="act"):
        nc = self.nc
        kw = {}
        ins = [in_]
        if bias is not None:
            kw["bias"] = _a(bias) if not isinstance(bias, (int, float)) else bias
            if not isinstance(bias, (int, float)):
                ins.append(bias)
        if scale is not None:
            kw["scale"] = _a(scale) if not isinstance(scale, (int, float)) else scale
            if not isinstance(scale, (int, float)):
                ins.append(scale)
        return self.p.op("act", lambda: nc.scalar.activation(_a(out), _a(in_), func, **kw), ins, [out])

    def tt(self, e, out, in0, in1, op):
        eng = self.p.eng[e]
        return self.p.op(e, lambda: eng.tensor_tensor(_a(out), _a(in0), _a(in1), op), [in0, in1], [out])

    def ts(self, e, out, in0, s1, s2=None, op0=ALU.mult, op1=None):
        eng = self.p.eng[e]
        ins = [in0]
        a1 = s1
        if not isinstance(s1, (int, float)):
            ins.append(s1); a1 = _a(s1)
        a2 = s2
        if s2 is not None and not isinstance(s2, (int, float)):
            ins.append(s2); a2 = _a(s2)
        if op1 is None:
            return self.p.op(e, lambda: eng.tensor_scalar(_a(out), _a(in0), a1, None, op0), ins, [out])
        return self.p.op(e, lambda: eng.tensor_scalar(_a(out), _a(in0), a1, a2, op0, op1), ins, [out])

    def stt(self, out, in0, scalar, in1, op0, op1, accum_out=None):
        nc = self.nc
        ins = [in0, in1]
        a = scalar
        if not isinstance(scalar, (int, float)):
            ins.append(scalar); a = _a(scalar)
        if accum_out is not None:
            return self.p.op("dve", lambda: nc.vector.scalar_tensor_tensor(_a(out), _a(in0), a, _a(in1), op0, op1, accum_out=_a(accum_out)),
                             ins, [out, accum_out])
        return self.p.op("dve", lambda: nc.vector.scalar_tensor_tensor(_a(out), _a(in0), a, _a(in1), op0, op1), ins, [out])

    def cp(self, e, out, in_):
        eng = self.p.eng[e]
        if e == "act":
            return self.p.op(e, lambda: eng.copy(_a(out), _a(in_)), [in_], [out])
        return self.p.op(e, lambda: eng.tensor_copy(_a(out), _a(in_)), [in_], [out])

    def memset(self, e, out, val):
        eng = self.p.eng[e]
        return self.p.op(e, lambda: eng.memset(_a(out), val), [], [out])

    def recip(self, out, in_):
        nc = self.nc
        return self.p.op("dve", lambda: nc.vector.reciprocal(_a(out), _a(in_)), [in_], [out])

    def ld(self, out, in_, q="sp", **kw):
        return self.p.dma(q, out, in_, **kw)


def _a(x):
    return x[0] if isinstance(x, tuple) else x


def K(ap, key):
    return (ap, key)


WNAMES = [("w_ada", (2, 1024, 6144)), ("b_ada", (2, 6144)), ("w_in", (2, 1024, INW)),
          ("hg_lb_logits", (2, 2, 1024)), ("hg_norm_g", (2, 128)), ("w_hg_o", (2, 1024, 1024)),
          ("conv_dw", (2, 31, 1024)), ("conv_b", (2, 1024)), ("conv_ln_g", (2, 1024)),
          ("conv_ln_b", (2, 1024)), ("w_conv_o", (2, 1024, 1024)), ("att_qn_g", (2, 128)),
          ("att_kn_g", (2, 128)), ("w_att_o", (2, 1024, 1024)), ("w_out", (2, 1024, 1024)),
          ("ln1_g", (2, 1024)), ("ln1_b", (2, 1024)), ("peer_wq", (2, 1024, 2048)),
          ("peer_k1", (2, 128, 128)), ("peer_k2", (2, 128, 128)), ("peer_u", (2, 16384, 1024)),
          ("peer_v", (2, 16384, 1024)), ("ln2_g", (2, 1024)), ("ln2_b", (2, 1024))]


def host_consts():
    c = {}
    c["ident"] = np.eye(128, dtype=np.float32)
    s = np.arange(128)[:, None]
    t = np.arange(128)[None, :]
    same = (s // CH) == (t // CH)
    c["maskf"] = (same & (t >= s)).astype(np.float32)
    c["maskb"] = (same & (t <= s)).astype(np.float32)
    n_freq = 32
    inv = (10000.0 ** (-np.arange(n_freq, dtype=np.float32) / np.float32(n_freq))).astype(np.float32)
    row = np.repeat(np.arange(64), 64).astype(np.float32)
    col = np.tile(np.arange(64), 64).astype(np.float32)
    ang = np.concatenate([row[:, None] * inv, col[:, None] * inv], axis=-1).astype(np.float32)
    c["ropecs_full"] = np.concatenate([np.cos(ang), np.sin(ang)], axis=-1).astype(np.float32)
    c["iota256"] = np.tile(np.arange(256, dtype=np.float32)[None, :], (128, 1))
    return c


DBG_SHAPES = {"DBG_A": (128, 8352), "DBG_B": (1024, NT), "DBG_P": (NT, 1024), "DBG_E": (NT, 128), "DBG_W": (NT, 128), "DBG_ACT": (NT, 128), "DBG_SC": (NT, 2048), "DBG_V1": (NT, 256), "DBG_I1": (NT, 256), "DBG_HH": (NT, 1024), "DBG_SCO": (NT, 128), "DBG_CI": (NT, 128)}
CONST_SHAPES = {"ident": (128, 128), "maskf": (128, 128), "maskb": (128, 128),
                "ropecs": (NX, 128), "iota256": (128, 256), "rolem": (128, 2)}


def build(nlayers=DEPTH, stop_after=None, debug=()):
    b = B(debug)
    nc, p, dr = b.nc, b.p, b.dr
    b.inp("xin", (NT, D))
    b.inp("cvec", (16, 128))
    for n, s in WNAMES:
        b.inp(n, s)
    for n, s in CONST_SHAPES.items():
        b.inp(n, s)
    b.outp("y", (NX, D))
    for n, shp in DBG_SHAPES.items():
        if n in b.debug:
            b.outp(n, shp, BF16 if n == "DBG_B" else F32)
    for n in ("XC", "X1"):
        b.scr(n, (NT, D))
    for n in ("VHG", "KTOKf", "KTOKb"):
        b.scr(n, (NT, D), BF16)
    for n in ("OHGf", "OHGb"):
        b.scr(n, (1024, NT))
    for n in ("QTf", "KTf", "QTb", "KTb"):
        b.scr(n, (1024, NT), BF16)
    for n in ("DLf", "DLb"):
        b.scr(n, (1024, NCH))
    for n in ("GT", "UT", "QTA", "YHG", "YCONV", "YATT"):
        b.scr(n, (1024, NT), BF16)
    b.scr("MG", (3072, NT), BF16)
    b.scr("KTA", (512, NCTX), BF16)
    b.scr("VATT", (NCTX, 512), BF16)
    b.scr("KXH", (512, NX), BF16)
    b.scr("VXH", (NX, 512), BF16)
    b.scr("KXF", (1024, NX), BF16)
    b.scr("VXF", (2 * NX, 512), BF16)
    b.scr("UE", (1024, 32), BF16)
    b.scr("UEF", (2048, 32), BF16)
    b.scr("UV16", (2 * 16384, 2048), BF16)
    b.scr("SND", (2048, 128))
    b.scr("RCV", (4096, 128))

    with ExitStack() as glob:
        def sb(name, shape, dt=F32, es=glob):
            return es.enter_context(nc.sbuf_tensor(name, list(shape), dt))

        def ps(name, shape, dt=F32, es=glob):
            return es.enter_context(nc.psum_tensor(name, list(shape), dt))

        ident = sb("ident_sb", (128, 128))
        identb = sb("identb_sb", (128, 128), BF16)
        ones32 = sb("ones32", (128, 128))
        onesb = sb("onesb", (128, 128), BF16)
        epsc = sb("epsc", (128, 1))
        rolem = sb("rolem_sb", (128, 2))
        b.ld(rolem[:], dr["rolem"])
        b.ld(ident[:], dr["ident"])
        b.cp("dve", identb[:], ident[:])
        b.memset("dve", ones32[:], 1.0)
        b.memset("dve", onesb[:], 1.0)
        b.memset("dve", epsc[:], EPS)
        MODP = sb("MODP", (128, 2, 2, 8))
        MODBC = sb("MODBC", (128, 2, 4, 1024))
        VT = sb("VT", (128, 80))
        LB = sb("LB", (128, 3, 16))
        st = dict(b=b, sb=sb, ps=ps, ident=ident, identb=identb, ones32=ones32, onesb=onesb,
                  epsc=epsc, rolem=rolem, MODP=MODP, MODBC=MODBC, VT=VT, LB=LB)
        p.barrier()
        if st.get("nph", 7) >= 7 and stop_after is None or (stop_after is not None and stop_after[1] == "phase_i"):
            phase_tab(st)
            p.barrier()
        for l in range(nlayers):
            last = l == DEPTH - 1
            src = dr["xin"] if l == 0 else dr["XC"]
            dst = dr["y"] if last else dr["XC"]
            phases = [phase_a, phase_bcd, phase_e, phase_f, phase_g, phase_h, phase_i][:st.get("nph", 7)]
            for ph in phases:
                ph(st, l, last, src, dst)
                p.barrier()
                if stop_after == (l, ph.__name__):
                    break
            else:
                continue
            break
        p.barrier()
    return b


GROUPS = [(0, 256)] + [(256 + 512 * i, 512) for i in range(NX // 512)]


def phase_a(st, l, last, src, dst):
    b = st["b"]; nc, p, dr = b.nc, b.p, b.dr
    ident, MODP, MODBC, VT, LB = st["ident"], st["MODP"], st["MODBC"], st["VT"], st["LB"]
    with ExitStack() as es:
        sb = lambda n, s, dt=F32: st["sb"](n + f"_a{l}", s, dt, es)
        ps = lambda n, s, dt=F32: st["ps"](n + f"_a{l}", s, dt, es)
        stg = sb("stg", (80, 128))
        b.ld(stg[0:16, :], dr["cvec"])
        b.ld(stg[16:48, :], dr["hg_lb_logits"].rearrange("l d (h p) -> (l d h) p", p=128))
        b.ld(stg[48:56, :], dr["conv_b"][l].rearrange("(k p) -> k p", p=128))
        b.ld(stg[56:64, :], dr["conv_ln_g"][l].rearrange("(k p) -> k p", p=128))
        b.ld(stg[64:72, :], dr["conv_ln_b"][l].rearrange("(k p) -> k p", p=128))
        b.ld(stg[72:73, :], dr["hg_norm_g"][l:l + 1, :])
        pT = ps("pT", (128, 512))
        b.tr(pT[:, 0:73], stg[0:73, :], ident[0:73, 0:73])
        b.cp("dve", VT[:, 0:73], pT[:, 0:73])
        if l == 0:
            b.memset("dve", LB[:, 0, :], 0.0)
        else:
            dlt = sb("dlt", (128, 16))
            b.tt("dve", dlt[:], VT[:, 16:32], VT[:, 32:48], ALU.subtract)
            b.act(LB[:, 0, :], dlt[:], AF.Sigmoid)
        b.ts("dve", LB[:, 1, :], LB[:, 0, :], -1.0, 1.0, ALU.mult, ALU.add)
        b.ts("dve", LB[:, 2, :], LB[:, 0, :], -1.0, None, ALU.add)
        csil = sb("csil", (128, 16))
        b.act(csil[:], VT[:, 0:16], AF.Silu)
        crep = sb("crep", (128, 16, 128))
        for j in range(16):
            b.cp("dve", K(crep[:, j, :], f"crep{j}"), csil[:, j:j + 1].to_broadcast([128, 128]))
        wad = [sb(f"wad{i}", (128, 8, 512)) for i in range(2)]
        bia = [sb(f"bia{i}", (128, 512)) for i in range(2)]
        tmp = [sb(f"tmpa{i}", (128, 512)) for i in range(2)]
        pA = [ps(f"pA{i}", (128, 512)) for i in range(2)]
        for n in range(12):
            w = wad[n % 2]; bi = bia[n % 2]
            b.ld(w[:], dr["w_ada"][l, :, n * 512:(n + 1) * 512].rearrange("(k p) c -> p k c", p=128))
            b.ld(bi[:], dr["b_ada"][l, n * 512:(n + 1) * 512].partition_broadcast(128), q="pool")
            idx6, half = n // 2, n % 2
            for v in range(2):
                for k in range(8):
                    b.mm(pA[v][:], K(crep[:, v * 8 + k, :], f"crep{v * 8 + k}"), w[:, k, :], start=(k == 0), stop=(k == 7))
                t = tmp[v]
                b.tt("dve", t[:], pA[v][:], bi[:], ALU.add)
                hs = slice(half * 512, (half + 1) * 512)
                if idx6 in (0, 1):
                    for j in range(4):
                        b.tr(pT[:, j * 128:(j + 1) * 128], t[:, j * 128:(j + 1) * 128], ident[:])
                    for j in range(4):
                        o = MODP[:, v, idx6, half * 4 + j:half * 4 + j + 1]
                        if idx6 == 0:
                            b.cp("dve", o, pT[:, j * 128:j * 128 + 1])
                        else:
                            b.ts("dve", o, pT[:, j * 128:j * 128 + 1], 1.0, None, ALU.add)
                elif idx6 == 2:
                    b.cp("act", MODBC[:, v, 0, hs], t[:])
                elif idx6 == 3:
                    b.cp("act", MODBC[:, v, 2, hs], t[:])
                elif idx6 == 4:
                    b.ts("dve", MODBC[:, v, 3, hs], t[:], 1.0, None, ALU.add)
                else:
                    b.cp("act", MODBC[:, v, 1, hs], t[:])
        if "DBG_A" in b.debug:
            b.ld(dr["DBG_A"][:, 0:8192], MODBC[:].rearrange("p a b c -> p (a b c)"), q="pool")
            b.ld(dr["DBG_A"][:, 8192:8224], MODP[:].rearrange("p a b c -> p (a b c)"), q="pool")
            b.ld(dr["DBG_A"][:, 8224:8304], VT[:], q="pool")
            b.ld(dr["DBG_A"][:, 8304:8352], LB[:].rearrange("p a b -> p (a b)"), q="pool")
        p.barrier()


def phase_bcd(st, l, last, src, dst):
    b = st["b"]; nc, p, dr = b.nc, b.p, b.dr
    ident, identb, MODP, LB = st["ident"], st["identb"], st["MODP"], st["LB"]
    with ExitStack() as es0:
        xmodT = st["sb"](f"xmodT{l}", (128, 8, NT), BF16, es0)
        with ExitStack() as es:
            sb = lambda n, s, dt=F32: st["sb"](n + f"_b{l}", s, dt, es)
            ps = lambda n, s, dt=F32: st["ps"](n + f"_b{l}", s, dt, es)
            xt = [sb(f"xt{i}", (128, D)) for i in range(3)]
            pB = [ps(f"pB{i}", (128, 512)) for i in range(4)]
            cnt = 0
            for i in range(NTILE):
                v = 1 if i < 2 else 0
                x_ = xt[i % 3]
                b.ld(x_[:], src[i * 128:(i + 1) * 128, :])
                for hf in range(2):
                    pb = pB[(2 * i + hf) % 4]
                    for j in range(4):
                        k = hf * 4 + j
                        b.tr(pb[:, j * 128:(j + 1) * 128], x_[:, k * 128:(k + 1) * 128], ident[:])
                    for j in range(4):
                        k = hf * 4 + j
                        o = K(xmodT[:, k, i * 128:(i + 1) * 128], f"xmodT:{i}")
                        if cnt % 2 == 0:
                            b.act(o, pb[:, j * 128:(j + 1) * 128], AF.Identity,
                                  bias=MODP[:, v, 0, k:k + 1], scale=MODP[:, v, 1, k:k + 1])
                        else:
                            b.ts("dve", o, pb[:, j * 128:(j + 1) * 128], MODP[:, v, 1, k:k + 1],
                                 MODP[:, v, 0, k:k + 1], ALU.mult, ALU.add)
                        cnt += 1
            if "DBG_B" in b.debug:
                b.ld(dr["DBG_B"].rearrange("(k p) t -> p k t", p=128), xmodT[:], q="pool")
            p.barrier()
        xm = "xmodT_ro"

        def proj_fm(pst, wbf, t0, n):
            for k in range(8):
                b.mm(pst[:, 0:n], wbf[:, k, :], K(xmodT[:, k, t0:t0 + n], xm), start=(k == 0), stop=(k == 7))

        with ExitStack() as es:
            sb = lambda n, s, dt=F32: st["sb"](n + f"_c{l}", s, dt, es)
            ps = lambda n, s, dt=F32: st["ps"](n + f"_c{l}", s, dt, es)
            wtmp = [sb(f"wtmp{i}", (128, 8, 128)) for i in range(3)]
            wbf = [sb(f"wbf{i}", (128, 8, 128), BF16) for i in range(6)]
            wc = [0]

            def loadw(col0):
                i = wc[0]; wc[0] += 1
                wt = wtmp[i % 3]; wb = wbf[i % 6]
                b.ld(wt[:], dr["w_in"][l, :, col0:col0 + 128].rearrange("(k p) c -> p k c", p=128))
                b.cp("pool", wb[:], wt[:])
                return wb

            pC = [ps(f"pC{i}", (128, 512)) for i in range(6)]
            pT = [ps(f"pT{i}", (128, 1024), BF16) for i in range(2)]
            qf = [sb(f"qf{i}", (128, 512)) for i in range(2)]
            tb = {}
            for d in range(2):
                for nm in ("sg", "lf", "bp", "kk", "tm", "E", "Ei"):
                    tb[nm, d] = sb(f"{nm}{d}", (128, 512))
                for nm in ("qt", "kt", "kh"):
                    for par in range(2):
                        tb[nm, d, par] = sb(f"{nm}{d}{par}", (128, 512), BF16)
                for par in range(2):
                    tb["ktok", d, par] = sb(f"ktok{d}{par}", (128, 4, 128), BF16)
                    tb["dl", d, par] = sb(f"dl{d}{par}", (128, 32))
            jobs = [(h, t0, n) for h in range(8) for (t0, n) in GROUPS]
            wts = {}

            def proj_job(ji):
                h, t0, n = jobs[ji]
                if h not in wts:
                    wts[h] = (loadw(C_HGQ + h * 128), loadw(C_HGFF + h * 128), loadw(C_HGFB + h * 128))
                par = ji % 2
                proj_fm(pC[3 * par], wts[h][0], t0, n)
                proj_fm(pC[3 * par + 1], wts[h][1], t0, n)
                proj_fm(pC[3 * par + 2], wts[h][2], t0, n)

            def chain1(ji, d):
                h, t0, n = jobs[ji]
                par = ji % 2; nck = n // CH
                pz = pC[3 * par + 1 + d]
                q_ = qf[par]
                sg, lf, bp, kk, tm, E, Ei = (tb[nm, d] for nm in ("sg", "lf", "bp", "kk", "tm", "E", "Ei"))
                qt, kt, kh, ktok, dl = (tb[nm, d, par] for nm in ("qt", "kt", "kh", "ktok", "dl"))
                ci = d * 8 + h
                b.act(sg[:, 0:n], pz[:, 0:n], AF.Sigmoid); yield
                b.ts("pool", kk[:, 0:n], sg[:, 0:n], LB[:, 2, ci:ci + 1], LB[:, 1, ci:ci + 1], ALU.mult, ALU.add); yield
                b.ts("dve", tm[:, 0:n], sg[:, 0:n], LB[:, 1, ci:ci + 1], LB[:, 0, ci:ci + 1], ALU.mult, ALU.add); yield
                b.act(lf[:, 0:n], tm[:, 0:n], AF.Ln); yield
                srcb = lf
                for si, sh_ in enumerate((1, 2, 4, 8)):
                    dstb = tm if si % 2 == 0 else bp
                    sv = srcb[:, 0:n].rearrange("p (c s) -> p c s", s=CH)
                    dv = dstb[:, 0:n].rearrange("p (c s) -> p c s", s=CH)
                    b.tt("dve", dv[:, :, sh_:], sv[:, :, sh_:], sv[:, :, :CH - sh_], ALU.add); yield
                    b.cp("pool", dv[:, :, 0:sh_], sv[:, :, 0:sh_]); yield
                    srcb = dstb
                bl = bp[:, CH - 1:n:CH]
                if d == 0:
                    bb = bp
                else:
                    b.tt("dve", tm[:, 0:n], lf[:, 0:n], bp[:, 0:n], ALU.subtract); yield
                    b.tt("dve", tm[:, 0:n].rearrange("p (c s) -> p c s", s=CH),
                         tm[:, 0:n].rearrange("p (c s) -> p c s", s=CH),
                         bp[:, 0:n].rearrange("p (c s) -> p c s", s=CH)[:, :, CH - 1:CH].to_broadcast([128, nck, CH]),
                         ALU.add); yield
                    bb = tm
                b.act(dl[:, 0:nck], bl, AF.Exp); yield
                b.ld(dr["DLf" if d == 0 else "DLb"][h * 128:(h + 1) * 128, t0 // CH:t0 // CH + nck], dl[:, 0:nck], q="pool")
                b.ts("dve", E[:, 0:n], bb[:, 0:n], -80.0, None, ALU.max); yield
                b.act(Ei[:, 0:n], E[:, 0:n], AF.Exp, scale=-1.0); yield
                b.act(E[:, 0:n], E[:, 0:n], AF.Exp); yield
                b.tt("dve", qt[:, 0:n], q_[:, 0:n], E[:, 0:n], ALU.mult); yield
                b.tt("pool", kt[:, 0:n], kk[:, 0:n], Ei[:, 0:n], ALU.mult); yield
                dn = "f" if d == 0 else "b"
                b.ld(dr["QT" + dn][h * 128:(h + 1) * 128, t0:t0 + n], qt[:, 0:n], q="pool")
                b.ld(dr["KT" + dn][h * 128:(h + 1) * 128, t0:t0 + n], kt[:, 0:n], q="pool")
                b.tt("dve", lf[:, 0:n].rearrange("p (c s) -> p c s", s=CH),
                     bp[:, 0:n].rearrange("p (c s) -> p c s", s=CH)[:, :, CH - 1:CH].to_broadcast([128, nck, CH]),
                     bb[:, 0:n].rearrange("p (c s) -> p c s", s=CH), ALU.subtract); yield
                b.act(lf[:, 0:n], lf[:, 0:n], AF.Exp); yield
                b.tt("pool", kh[:, 0:n], kk[:, 0:n], lf[:, 0:n], ALU.mult); yield

            def chain2(ji, d):
                h, t0, n = jobs[ji]
                par = ji % 2
                kh, ktok = tb["kh", d, par], tb["ktok", d, par]
                dn = "f" if d == 0 else "b"
                pt = pT[d]
                for j in range(n // 128):
                    b.tr(pt[:, j * 128:(j + 1) * 128], kh[:, j * 128:(j + 1) * 128], identb[:])
                b.cp("act", ktok[:, 0:n // 128, :], pt[:, 0:n].rearrange("p (j c) -> p j c", c=128))
                b.ld(dr["KTOK" + dn][t0:t0 + n, h * 128:(h + 1) * 128].rearrange("(j p) c -> p j c", p=128),
                     ktok[:, 0:n // 128, :], q="pool")

            proj_job(0)
            for ji, (h, t0, n) in enumerate(jobs):
                par = ji % 2
                b.act(qf[par][:, 0:n], pC[3 * par][:, 0:n], AF.Silu)
                gens = [chain1(ji, 0), chain1(ji, 1)]
                while gens:
                    for g_ in list(gens):
                        if next(g_, "done") == "done":
                            gens.remove(g_)
                if ji + 1 < len(jobs):
                    proj_job(ji + 1)
                chain2(ji, 0); chain2(ji, 1)
            job = len(jobs)
            ob = [sb(f"ob{i}", (128, 512), BF16) for i in range(3)]
            sgl = [sb(f"sgl{i}", (128, 512)) for i in range(2)]
            oc = 0
            for c in range(8):
                wg = loadw(C_HGG + c * 128)
                wa = loadw(C_GLUA + c * 128)
                wgg = loadw(C_GLUG + c * 128)
                for (t0, n) in GROUPS:
                    par = job % 2; job += 1
                    pg, pa, pgg = pC[3 * par], pC[3 * par + 1], pC[3 * par + 2]
                    proj_fm(pg, wg, t0, n)
                    proj_fm(pa, wa, t0, n)
                    proj_fm(pgg, wgg, t0, n)
                    o = ob[oc % 3]; oc += 1
                    b.act(o[:, 0:n], pg[:, 0:n], AF.Silu)
                    b.ld(dr["GT"][c * 128:(c + 1) * 128, t0:t0 + n], o[:, 0:n], q="pool")
                    s_ = sgl[par]
                    b.act(s_[:, 0:n], pgg[:, 0:n], AF.Sigmoid)
                    o = ob[oc % 3]; oc += 1
                    b.tt("dve", o[:, 0:n], pa[:, 0:n], s_[:, 0:n], ALU.mult)
                    b.ld(dr["UT"][c * 128:(c + 1) * 128, t0:t0 + n], o[:, 0:n], q="pool")
            for c in range(24):
                wm = loadw(C_MG + c * 128)
                for (t0, n) in GROUPS:
                    pm = pC[job % 6]; job += 1
                    proj_fm(pm, wm, t0, n)
                    o = ob[oc % 3]; oc += 1
                    b.act(o[:, 0:n], pm[:, 0:n], AF.Sigmoid)
                    b.ld(dr["MG"][c * 128:(c + 1) * 128, t0:t0 + n], o[:, 0:n], q="pool")
            p.barrier()
        if st.get("skip_d"):
            return
        phase_d(st, l, last, xmodT, xm)


def phase_d(st, l, last, xmodT, xm):
    b = st["b"]; nc, p, dr = b.nc, b.p, b.dr
    ident, identb = st["ident"], st["identb"]
    with ExitStack() as es:
        sb = lambda n, s, dt=F32: st["sb"](n + f"_d{l}", s, dt, es)
        ps = lambda n, s, dt=F32: st["ps"](n + f"_d{l}", s, dt, es)
        WD = sb("WD", (128, 8, 3072), BF16)
        wst = [sb(f"wst{i}", (128, 8, 256)) for i in range(1)]
        cols = [C_HGI, C_HGI + 512, C_ATQ, C_ATQ + 512, C_ATK, C_ATV]
        for n_, c0 in enumerate(cols):
            for hh in range(2):
                w = wst[0]
                b.ld(w[:], dr["w_in"][l, :, c0 + hh * 256:c0 + hh * 256 + 256].rearrange("(k p) c -> p k c", p=128))
                b.cp("pool", K(WD[:, :, n_ * 512 + hh * 256:n_ * 512 + hh * 256 + 256], f"WD{n_}"), w[:])
        gq = sb("gq", (128, 128)); gk = sb("gk", (128, 128))
        b.ld(gq[:], dr["att_qn_g"][l].partition_broadcast(128), q="pool")
        b.ld(gk[:], dr["att_kn_g"][l].partition_broadcast(128), q="pool")
        pD = [ps(f"pD{i}", (128, 512)) for i in range(6)]
        pTq = ps("pTq", (128, 1024), BF16)
        pTk = ps("pTk", (128, 1024), BF16)
        vst = [sb(f"vst{i}", (128, 1024), BF16) for i in range(2)]
        vat = [sb(f"vat{i}", (128, 512), BF16) for i in range(2)]
        junk = sb("junk", (128, 128))
        ssq = [sb(f"ssq{i}", (128, 12)) for i in range(2)]
        qn = [sb(f"qn{i}", (128, 12, 128)) for i in range(1)] * 2
        qr = [sb(f"qr{i}", (128, 12, 128), BF16) for i in range(2)]
        cs = [sb(f"cs{i}", (128, 128)) for i in range(2)]
        rt = [sb(f"rt{i}", (128, 12, 64)) for i in range(4)]
        qTs = [sb(f"qTs{i}", (128, 8, 128), BF16) for i in range(2)]
        kTs = [sb(f"kTs{i}", (128, 4, 128), BF16) for i in range(2)]
        def mm_tile(i):
            t0 = i * 128
            for n_ in range(6):
                for k in range(8):
                    b.mm(pD[n_][:], K(xmodT[:, k, t0:t0 + 128], xm), K(WD[:, k, n_ * 512:(n_ + 1) * 512], f"WD{n_}"),
                         start=(k == 0), stop=(k == 7))

        mm_tile(0)
        for i in range(NTILE):
            par = i % 2
            t0 = i * 128
            v_ = vst[par]
            b.cp("act", v_[:, 0:512], pD[0][:])
            b.cp("act", v_[:, 512:1024], pD[1][:])
            b.ld(dr["VHG"][t0:t0 + 128, :], v_[:], q="pool")
            va = vat[par]
            b.cp("act", va[:], pD[5][:])
            if i < 2:
                b.ld(dr["VATT"][t0:t0 + 128, :], va[:], q="pool")
            else:
                b.ld(dr["VXH"][t0 - NCTX:t0 - NCTX + 128, :], va[:], q="pool")
            s_ = ssq[par]
            q_ = qn[par]
            for jj in range(3):
                b.cp("act", q_[:, jj * 4:(jj + 1) * 4, :], pD[2 + jj][:].rearrange("p (h c) -> p h c", c=128))
            hp = lambda j: q_[:, j, :]
            for j in range(12):
                b.stt(junk[:], hp(j), 1.0, hp(j), ALU.mult, ALU.mult, accum_out=s_[:, j:j + 1])
            b.ts("dve", s_[:], s_[:], 1.0 / 128.0, EPS, ALU.mult, ALU.add)
            b.act(s_[:], s_[:], AF.Sqrt)
            b.recip(s_[:], s_[:])
            for j in range(12):
                b.stt(q_[:, j, :], hp(j), s_[:, j:j + 1], (gq if j < 8 else gk)[:], ALU.mult, ALU.mult)
            r_ = qr[par]
            if i >= 2:
                c_ = cs[par]
                b.ld(c_[:], dr["ropecs"][(i - 2) * 128:(i - 1) * 128, :])
                x0 = q_[:, :, 0:128:2]; x1 = q_[:, :, 1:128:2]
                cosb = c_[:, None, 0:64].to_broadcast([128, 12, 64])
                sinb = c_[:, None, 64:128].to_broadcast([128, 12, 64])
                t1, t2, t3, t4 = rt
                b.tt("dve", t1[:], x0, cosb, ALU.mult)
                b.tt("pool", t2[:], x1, sinb, ALU.mult)
                b.tt("dve", t3[:], x0, sinb, ALU.mult)
                b.tt("pool", t4[:], x1, cosb, ALU.mult)
                b.tt("dve", r_[:, :, 0:128:2], t1[:], t2[:], ALU.subtract)
                b.tt("pool", r_[:, :, 1:128:2], t3[:], t4[:], ALU.add)
            else:
                b.cp("dve", r_[:], q_[:])
            if i + 1 < NTILE:
                mm_tile(i + 1)
            for j in range(8):
                b.tr(pTq[:, j * 128:(j + 1) * 128], r_[:, j, :], identb[:])
            for j in range(4):
                b.tr(pTk[:, j * 128:(j + 1) * 128], r_[:, 8 + j, :], identb[:])
            qs, ks = qTs[par], kTs[par]
            b.cp("act", qs[:], pTq[:].rearrange("p (h t) -> p h t", t=128))
            b.cp("dve", ks[:], pTk[:, 0:512].rearrange("p (h t) -> p h t", t=128))
            b.ld(dr["QTA"][:, t0:t0 + 128].rearrange("(h p) t -> p h t", p=128), qs[:], q="pool")
            if i < 2:
                b.ld(dr["KTA"][:, t0:t0 + 128].rearrange("(h p) t -> p h t", p=128), ks[:], q="pool")
            else:
                b.ld(dr["KXH"][:, t0 - NCTX:t0 - NCTX + 128].rearrange("(h p) t -> p h t", p=128), ks[:], q="pool")
        ues = sb("ues", (128, 8, 32), BF16)
        b.memset("pool", ues[:], 0.0)
        b.ld(ues[:, :, 0:15], dr["UT"][:, NCTX:NCTX + 15].rearrange("(c p) t -> p c t", p=128))
        b.ld(ues[:, :, 16:31], dr["UT"][:, NT - 15:NT].rearrange("(c p) t -> p c t", p=128))
        b.ld(dr["UE"].rearrange("(c p) t -> p c t", p=128), ues[:], q="pool")
        p.barrier()
        p.allgather_pairs(dr["KXH"], dr["KXF"])
        p.allgather_pairs(dr["VXH"], dr["VXF"])
        p.allgather_pairs(dr["UE"], dr["UEF"])
        p.barrier()


def phase_e(st, l, last, src, dst):
    b = st["b"]; nc, p, dr = b.nc, b.p, b.dr
    ident, ones32, VT, epsc = st["ident"], st["ones32"], st["VT"], st["epsc"]
    eso = ExitStack()
    SCT = [st["sb"](f"SCT{d}_{l}", (128, 8, 128), F32, eso) for d in range(2)]
    with ExitStack() as es:
        sb = lambda n, s, dt=F32: st["sb"](n + f"_e{l}", s, dt, es)
        ps = lambda n, s, dt=F32: st["ps"](n + f"_e{l}", s, dt, es)
        mask = [sb("maskf", (128, 128)), sb("maskb", (128, 128))]
        b.ld(mask[0][:], dr["maskf"]); b.ld(mask[1][:], dr["maskb"])
        S32 = [sb(f"S32_{d}", (128, 8, 128)) for d in range(2)]
        S16 = [sb(f"S16_{d}", (128, 8, 128), BF16) for d in range(2)]
        for d in range(2):
            b.memset("pool", S32[d][:], 0.0)
            b.memset("pool", S16[d][:], 0.0)
        Sk = lambda d, h: K(S32[d][:, h, :], f"S{d}{h}")
        Tk = lambda d, h: K(S16[d][:, h, :], f"T{d}{h}")
        qT = [[sb(f"qT{d}{i}", (128, 8, 128), BF16) for i in range(2)] for d in range(2)]
        kT = [[sb(f"kT{d}{i}", (128, 8, 128), BF16) for i in range(2)] for d in range(2)]
        kk = [[sb(f"kk{d}{i}", (128, 3, 1024), BF16) for i in range(2)] for d in range(2)]
        vv = [[sb(f"vv{d}{i}", (128, 3, 1024), BF16) for i in range(2)] for d in range(2)]
        vf = [[sb(f"vf{d}{i}", (128, 1024), BF16) for i in range(2)] for d in range(2)]
        dl = [[sb(f"dl{d}{i}", (128, 8, 8)) for i in range(2)] for d in range(2)]
        scm = [sb(f"scm{i}", (128, 128), BF16) for i in range(4)]
        ost2 = [[sb(f"ost{d}{i}", (128, 8, 128)) for i in range(2)] for d in range(2)]
        pO = [ps(f"pO{i}", (128, 1024)) for i in range(2)]
        pSt = [ps(f"pSt{i}", (128, 512)) for i in range(4)]
        pS = pSt[2:4]
        order = [list(range(NTILE)), [1, 0] + list(range(NTILE - 1, 1, -1))]
        nsc = 0; nst = 0
        for step in range(NTILE):
            par = step % 2
            for d in range(2):
                dn = "f" if d == 0 else "b"
                ti = order[d][step]; t0 = ti * 128
                b.ld(qT[d][par][:], dr["QT" + dn][:, t0:t0 + 128].rearrange("(h p) t -> p h t", p=128))
                b.ld(kT[d][par][:], dr["KT" + dn][:, t0:t0 + 128].rearrange("(h p) t -> p h t", p=128))
                b.ld(vf[d][par][:], dr["VHG"][t0:t0 + 128, :])
                b.ld(dl[d][par][:], dr["DL" + dn][:, t0 // CH:t0 // CH + 8].rearrange("(h p) c -> p h c", p=128))
                for j in range(8):
                    g, a = j % 3, j // 3
                    b.ld(K(kk[d][par][32 * g:32 * g + 16, a, :], kk[d][par].name), dr["KTOK" + dn][t0 + j * CH:t0 + (j + 1) * CH, :])
                    b.ld(K(vv[d][par][32 * g:32 * g + 16, a, :], vv[d][par].name), dr["VHG"][t0 + j * CH:t0 + (j + 1) * CH, :])
            ost = [ost2[0][par], ost2[1][par]]
            dh = [(d, h) for d in range(2) for h in range(8)]
            b.mm(pS[nsc % 2][:, 0:128], kT[0][par][:, 0, :], qT[0][par][:, 0, :])
            for ii, (d, h) in enumerate(dh):
                pq = pS[nsc % 2]; sm = scm[nsc % 4]; nsc += 1
                if ii + 1 < len(dh):
                    d2, h2 = dh[ii + 1]
                    b.mm(pS[nsc % 2][:, 0:128], kT[d2][par][:, h2, :], qT[d2][par][:, h2, :])
                b.tt("dve", sm[:], pq[:, 0:128], mask[d][:], ALU.mult)
                b.mm(K(pO[d][:, h * 128:(h + 1) * 128], f"pO{d}"), vf[d][par][:, h * 128:(h + 1) * 128], sm[:],
                     start=(h % 4 == 0), stop=False)
            for jj in range(8):
                for d in range(2):
                    j = jj if d == 0 else 7 - jj
                    g, a = j % 3, j // 3
                    for h in range(8):
                        b.mm(K(pO[d][:, h * 128 + j * CH:h * 128 + (j + 1) * CH], f"pO{d}"), Tk(d, h),
                             qT[d][par][:, h, j * CH:(j + 1) * CH], start=False, stop=True)
                        pt = pSt[nst % 4]; nst += 1
                        b.mm(pt[:, 0:128], kk[d][par][32 * g:32 * g + 16, a, h * 128:(h + 1) * 128],
                             vv[d][par][32 * g:32 * g + 16, a, h * 128:(h + 1) * 128], start=True, stop=True)
                        b.stt(Sk(d, h), Sk(d, h), dl[d][par][:, h, j:j + 1], pt[:, 0:128], ALU.mult, ALU.add)
                        b.act(Tk(d, h), Sk(d, h), AF.Copy)
            for d in range(2):
                b.cp("act" if d == 0 else "dve", ost[d][:], K(pO[d][:].rearrange("p (h t) -> p h t", t=128), f"pO{d}"))
            if step == 1:
                for d in range(2):
                    for h in range(8):
                        b.cp("pool", K(SCT[d][:, h, :], f"SCT{d}"), Sk(d, h))
            for d in range(2):
                dn = "f" if d == 0 else "b"
                ti = order[d][step]; t0 = ti * 128
                b.ld(dr["OHG" + dn][:, t0:t0 + 128].rearrange("(h p) t -> p h t", p=128), ost[d][:], q="pool")
        for d in range(2):
            b.p.dma("sp", dr["SND"][d * 1024:(d + 1) * 1024, :].rearrange("(h p) v -> p h v", p=128), S32[d][:],
                    extra_in=[f"S{d}{h}" for h in range(8)])
        p.barrier()
        p.allgather_pairs(dr["SND"], dr["RCV"])
        p.barrier()
    with ExitStack() as es:
        sb = lambda n, s, dt=F32: st["sb"](n + f"_e2{l}", s, dt, es)
        ps = lambda n, s, dt=F32: st["ps"](n + f"_e2{l}", s, dt, es)
        rolem = st["rolem"]
        DLT = [sb(f"DLT{d}", (128, 8, 128)) for d in range(2)]
        rows = [slice(0, 1024), slice(3072, 4096)]
        for d in range(2):
            b.ld(DLT[d][:], dr["RCV"][rows[d], :].rearrange("(h p) v -> p h v", p=128))
            b.tt("dve", DLT[d][:], DLT[d][:], K(SCT[d][:], f"SCT{d}"), ALU.subtract)
            b.ts("dve", DLT[d][:], DLT[d][:], rolem[:, d:d + 1], None, ALU.mult)
        NXC = NX // CH
        PC = [sb(f"PC{d}", (128, 8, NXC)) for d in range(2)]
        pa = sb("pca", (128, 8, NXC)); pb_ = sb("pcb", (128, 8, NXC))
        for d in range(2):
            dn = "f" if d == 0 else "b"
            b.ld(pa[:], dr["DL" + dn][:, NCTX // CH:NCH].rearrange("(h p) c -> p h c", p=128))
            srcb, dstb = pa, pb_
            sh_ = 1
            while sh_ < NXC:
                if d == 0:
                    b.tt("dve", dstb[:, :, sh_:], srcb[:, :, sh_:], srcb[:, :, :NXC - sh_], ALU.mult)
                    b.cp("pool", dstb[:, :, 0:sh_], srcb[:, :, 0:sh_])
                else:
                    b.tt("dve", dstb[:, :, :NXC - sh_], srcb[:, :, :NXC - sh_], srcb[:, :, sh_:], ALU.mult)
                    b.cp("pool", dstb[:, :, NXC - sh_:], srcb[:, :, NXC - sh_:])
                srcb, dstb = dstb, srcb
                sh_ *= 2
            if d == 0:
                b.cp("dve", PC[d][:, :, 1:], srcb[:, :, :NXC - 1])
                b.memset("pool", PC[d][:, :, 0:1], 1.0)
            else:
                b.cp("dve", PC[d][:, :, :NXC - 1], srcb[:, :, 1:])
                b.memset("pool", PC[d][:, :, NXC - 1:], 1.0)
        of = [sb(f"of{i}", (128, 512)) for i in range(2)]
        ob = [sb(f"ob{i}", (128, 512)) for i in range(2)]
        qc = [[sb(f"qc{d}{i}", (128, 512)) for i in range(2)] for d in range(2)]
        qcb = [[sb(f"qcb{d}{i}", (128, 512), BF16) for i in range(2)] for d in range(2)]
        gt = [sb(f"gt{i}", (128, 512), BF16) for i in range(2)]
        sq = [sb(f"sq{i}", (128, 512)) for i in range(2)]
        rs = [sb(f"rs{i}", (128, 512)) for i in range(2)]
        yo = [sb(f"yo{i}", (128, 512), BF16) for i in range(2)]
        pss = [ps(f"pss{i}", (128, 512)) for i in range(2)]
        pcr = [ps(f"pcr{i}", (128, 512)) for i in range(2)]
        job = 0
        for h in range(8):
            for (t0, n) in GROUPS:
                par = job % 2; job += 1
                rows_ = slice(h * 128, (h + 1) * 128)
                b.ld(of[par][:, 0:n], dr["OHGf"][rows_, t0:t0 + n])
                b.ld(ob[par][:, 0:n], dr["OHGb"][rows_, t0:t0 + n])
                b.ld(gt[par][:, 0:n], dr["GT"][rows_, t0:t0 + n])
                o = of[par]
                b.tt("dve", o[:, 0:n], of[par][:, 0:n], ob[par][:, 0:n], ALU.add)
                if t0 >= NCTX:
                    c0 = (t0 - NCTX) // CH
                    for d in range(2):
                        dn = "f" if d == 0 else "b"
                        q_ = qc[d][par]; qb_ = qcb[d][par]
                        b.ld(qb_[:, 0:n], dr["QT" + dn][rows_, t0:t0 + n])
                        qv = q_[:, 0:n].rearrange("p (c s) -> p c s", s=CH)
                        b.tt("pool", qv, qb_[:, 0:n].rearrange("p (c s) -> p c s", s=CH),
                             PC[d][:, h, c0:c0 + n // CH, None].to_broadcast([128, n // CH, CH]), ALU.mult)
                        b.mm(pcr[par][:, 0:n], DLT[d][:, h, :], q_[:, 0:n], start=(d == 0), stop=(d == 1))
                    b.tt("dve", o[:, 0:n], o[:, 0:n], pcr[par][:, 0:n], ALU.add)
                b.act(sq[par][:, 0:n], o[:, 0:n], AF.Square)
                b.mm(pss[par][:, 0:n], ones32[:], sq[par][:, 0:n])
                b.ts("dve", rs[par][:, 0:n], pss[par][:, 0:n], 1.0 / 128.0, EPS, ALU.mult, ALU.add)
                b.act(rs[par][:, 0:n], rs[par][:, 0:n], AF.Sqrt)
                b.recip(rs[par][:, 0:n], rs[par][:, 0:n])
                b.tt("dve", o[:, 0:n], o[:, 0:n], rs[par][:, 0:n], ALU.mult)
                b.stt(yo[par][:, 0:n], o[:, 0:n], VT[:, 72:73], gt[par][:, 0:n], ALU.mult, ALU.mult)
                b.ld(dr["YHG"][rows_, t0:t0 + n], yo[par][:, 0:n], q="pool")
        p.barrier()
    eso.close()


def phase_f(st, l, last, src, dst):
    b = st["b"]; nc, p, dr = b.nc, b.p, b.dr
    ident, ones32, VT, epsc = st["ident"], st["ones32"], st["VT"], st["epsc"]
    with ExitStack() as es:
        sb = lambda n, s, dt=F32: st["sb"](n + f"_f{l}", s, dt, es)
        ps = lambda n, s, dt=F32: st["ps"](n + f"_f{l}", s, dt, es)
        dwr = sb("dwr", (31, 1024))
        b.ld(dwr[:], dr["conv_dw"][l])
        dwT = sb("dwT", (128, 8, 31))
        pT = ps("pT", (128, 512))
        for c in range(8):
            b.tr(pT[:, c * 32:c * 32 + 31], dwr[:, c * 128:(c + 1) * 128], ident[0:31, 0:31])
        for c in range(8):
            b.cp("dve", dwT[:, c, :], pT[:, c * 32:c * 32 + 31])
        DG = sb("DG", (128, 248, 128), BF16)
        for c in range(8):
            for k in range(31):
                b.ts("dve" if (c * 31 + k) % 2 == 0 else "pool", K(DG[:, c * 31 + k, :], f"DG{c}"), ident[:], dwT[:, c, k:k + 1], None, ALU.mult)
        ub = [sb(f"ub{i}", (128, 8, 542), BF16) for i in range(2)]
        hal = [sb(f"hal{i}", (128, 8, 15), BF16) for i in range(2)]
        CV = sb("CV", (128, 8, 512)); SQ = sb("SQ", (128, 8, 512))
        ycs = [sb(f"ycs{i}", (128, 8, 512), BF16) for i in range(2)]
        mean = sb("mean", (128, 512)); msq = sb("msq", (128, 512)); rstd = sb("rstd", (128, 512))
        pc = [ps(f"pc{i}", (128, 512)) for i in range(4)]
        pss = ps("pss", (128, 512)); psq = ps("psq", (128, 512))
        npc = 0
        for gi, (t0, n) in enumerate(GROUPS):
            if last and t0 < NCTX:
                continue
            s0, s1 = (0, NCTX) if t0 < NCTX else (NCTX, NT)
            lo, hi = t0 - 15, t0 + n + 15
            clo, chi = max(lo, s0), min(hi, s1)
            u = ub[gi % 2]
            if clo != lo or chi != hi:
                b.memset("pool", u[:], 0.0)
            b.ld(u[:, :, clo - lo:chi - lo], dr["UT"][:, clo:chi].rearrange("(c p) t -> p c t", p=128))
            if t0 == NCTX:
                b.ld(hal[0][:], dr["UEF"][0:1024, 16:31].rearrange("(c p) t -> p c t", p=128))
                b.ts("dve", u[:, :, 0:15], hal[0][:], st["rolem"][:, 0:1], None, ALU.mult)
            if t0 + n == NT:
                b.ld(hal[1][:], dr["UEF"][1024:2048, 0:15].rearrange("(c p) t -> p c t", p=128))
                b.ts("dve", u[:, :, n + 15:n + 30], hal[1][:], st["rolem"][:, 1:2], None, ALU.mult)
            for c in range(8):
                pcc = pc[npc % 4]; npc += 1
                for k in range(31):
                    b.mm(pcc[:, 0:n], K(DG[:, c * 31 + k, :], f"DG{c}"), u[:, c, k:k + n], start=(k == 0), stop=(k == 30))
                b.act(K(CV[:, c, 0:n], f"CV{c}"), pcc[:, 0:n], AF.Identity, bias=VT[:, 48 + c:49 + c])
                b.act(K(SQ[:, c, 0:n], f"SQ{c}"), pcc[:, 0:n], AF.Square, bias=VT[:, 48 + c:49 + c])
            for c in range(8):
                b.mm(pss[:, 0:n], ones32[:], K(CV[:, c, 0:n], f"CV{c}"), start=(c == 0), stop=(c == 7))
            for c in range(8):
                b.mm(psq[:, 0:n], ones32[:], K(SQ[:, c, 0:n], f"SQ{c}"), start=(c == 0), stop=(c == 7))
            b.ts("dve", mean[:, 0:n], pss[:, 0:n], 1.0 / 1024.0, None, ALU.mult)
            b.tt("dve", msq[:, 0:n], mean[:, 0:n], mean[:, 0:n], ALU.mult)
            b.stt(rstd[:, 0:n], psq[:, 0:n], 1.0 / 1024.0, msq[:, 0:n], ALU.mult, ALU.subtract)
            b.act(rstd[:, 0:n], rstd[:, 0:n], AF.Sqrt, bias=epsc[:, 0:1])
            b.recip(rstd[:, 0:n], rstd[:, 0:n])
            y = ycs[gi % 2]
            for c in range(8):
                cv = K(CV[:, c, 0:n], f"CV{c}")
                b.tt("dve", cv, cv, mean[:, 0:n], ALU.subtract)
                b.tt("pool", cv, cv, rstd[:, 0:n], ALU.mult)
                b.act(y[:, c, 0:n], cv, AF.Silu, bias=VT[:, 64 + c:65 + c], scale=VT[:, 56 + c:57 + c])
            b.ld(dr["YCONV"][:, t0:t0 + n].rearrange("(c p) t -> p c t", p=128), y[:, :, 0:n], q="pool")
        p.barrier()


def phase_g(st, l, last, src, dst):
    b = st["b"]; nc, p, dr = b.nc, b.p, b.dr
    onesb = st["onesb"]
    with ExitStack() as es:
        sb = lambda n, s, dt=F32: st["sb"](n + f"_g{l}", s, dt, es)
        ps = lambda n, s, dt=F32: st["ps"](n + f"_g{l}", s, dt, es)
        NK = NCTX + NXF
        KTr = sb("KTr", (128, 4, NK), BF16)
        Vr = sb("Vr", (128, NKT, 512), BF16)
        for h in range(4):
            b.ld(K(KTr[:, h, 0:NCTX], "KTr"), dr["KTA"][h * 128:(h + 1) * 128, :])
            for r_ in range(2):
                b.ld(K(KTr[:, h, NCTX + r_ * NX:NCTX + (r_ + 1) * NX], "KTr"), dr["KXF"][r_ * 512 + h * 128:r_ * 512 + (h + 1) * 128, :])
        b.ld(K(Vr[:, 0:2, :], "Vr"), dr["VATT"].rearrange("(t p) f -> p t f", p=128))
        b.ld(K(Vr[:, 2:NKT, :], "Vr"), dr["VXF"].rearrange("(t p) f -> p t f", p=128))
        qg = [sb(f"qg{i}", (128, 8, 512), BF16) for i in range(2)]
        yat = [sb(f"yat{i}", (128, 8, 512), BF16) for i in range(2)]
        pex = [sb(f"pex{i}", (128, 512), BF16) for i in range(3)]
        rin = [sb(f"rin{i}", (128, 512)) for i in range(2)]
        psc = [ps(f"psc{i}", (128, 512)) for i in range(3)]
        pO = [ps(f"pO{i}", (128, 512)) for i in range(2)]
        pR = [ps(f"pR{i}", (128, 512)) for i in range(2)]
        scale = 128.0 ** -0.5
        items = []
        for gi, (t0, n) in enumerate(GROUPS):
            if t0 < NCTX:
                if last:
                    continue
                ktiles = [0, 1]
            else:
                ktiles = list(range(NKT))
            for h in range(8):
                for ki, kt in enumerate(ktiles):
                    items.append((gi, t0, n, h, kt, ki == 0, ki == len(ktiles) - 1))
        loaded = set()
        hidx = {}

        def emit_sc(i):
            gi, t0, n, h, kt, first, lastk = items[i]
            q = qg[gi % 2]
            if gi not in loaded:
                loaded.add(gi)
                b.ld(q[:, :, 0:n], dr["QTA"][:, t0:t0 + n].rearrange("(h p) t -> p h t", p=128))
            b.mm(psc[i % 3][:, 0:n], K(KTr[:, h // 2, kt * 128:(kt + 1) * 128], "KTr"), q[:, h, 0:n])

        for i in range(min(2, len(items))):
            emit_sc(i)
        for i, (gi, t0, n, h, kt, first, lastk) in enumerate(items):
            kv = h // 2
            if (gi, h) not in hidx:
                hidx[(gi, h)] = len(hidx)
            nh = hidx[(gi, h)]
            po = pO[nh % 2]; pr = pR[nh % 2]; ri = rin[nh % 2]
            y = yat[gi % 2]
            pe_ = pex[i % 3]
            b.act(pe_[:, 0:n], psc[i % 3][:, 0:n], AF.Exp, scale=scale)
            if i + 2 < len(items):
                emit_sc(i + 2)
            b.mm(po[:, 0:n], K(Vr[:, kt, kv * 128:(kv + 1) * 128], "Vr"), pe_[:, 0:n], start=first, stop=lastk)
            b.mm(pr[:, 0:n], onesb[:], pe_[:, 0:n], start=first, stop=lastk)
            if lastk:
                b.recip(ri[:, 0:n], pr[:, 0:n])
                b.tt("dve", y[:, h, 0:n], po[:, 0:n], ri[:, 0:n], ALU.mult)
                if h == 7:
                    b.ld(dr["YATT"][:, t0:t0 + n].rearrange("(h p) t -> p h t", p=128), y[:, :, 0:n], q="pool")
        p.barrier()


def ln_tile(b, r, xo, gbc, bbc, st6, mv, epsc):
    nc, p = b.nc, b.p
    p.op("dve", lambda: nc.vector.bn_stats(st6[:, 0, :], r[:, 0:512]), [r[:]], [st6[:]])
    p.op("dve", lambda: nc.vector.bn_stats(st6[:, 1, :], r[:, 512:1024]), [r[:]], [st6[:]])
    p.op("dve", lambda: nc.vector.bn_aggr(mv[:, 0:2], st6[:].rearrange("p a b -> p (a b)")), [st6[:]], [mv[:]])
    b.act(mv[:, 2:3], mv[:, 1:2], AF.Sqrt, bias=epsc[:, 0:1])
    b.recip(mv[:, 3:4], mv[:, 2:3])
    b.ts("dve", r[:], r[:], mv[:, 0:1], mv[:, 3:4], ALU.subtract, ALU.mult)
    b.tt("pool", r[:], r[:], gbc[:], ALU.mult)
    b.tt("pool", xo[:], r[:], bbc[:], ALU.add)


def phase_h(st, l, last, src, dst):
    b = st["b"]; nc, p, dr = b.nc, b.p, b.dr
    MODBC, epsc = st["MODBC"], st["epsc"]
    with ExitStack() as es:
        sb = lambda n, s, dt=F32: st["sb"](n + f"_h{l}", s, dt, es)
        ps = lambda n, s, dt=F32: st["ps"](n + f"_h{l}", s, dt, es)
        wst = sb("wst", (128, 8, 256))
        W = []
        for wi, wn in enumerate(("w_hg_o", "w_conv_o", "w_att_o", "w_out")):
            w = sb(f"W{wi}", (128, 8, 1024), BF16)
            for q4 in range(4):
                b.ld(wst[:], dr[wn][l, :, q4 * 256:(q4 + 1) * 256].rearrange("(k p) c -> p k c", p=128))
                b.cp("pool" if q4 % 2 else "dve", w[:, :, q4 * 256:(q4 + 1) * 256], wst[:])
            W.append(w)
        lng = sb("lng", (128, 1024)); lnb = sb("lnb", (128, 1024))
        b.ld(lng[:], dr["ln1_g"][l].partition_broadcast(128), q="pool")
        b.ld(lnb[:], dr["ln1_b"][l].partition_broadcast(128), q="pool")
        Y = [sb(f"Y{i}", (128, 8, 512), BF16) for i in range(3)]
        MGs = sb("MGs", (128, 24, 512), BF16)
        mrg = sb("mrg", (128, 8, 512), BF16)
        tm = [sb(f"tm{i}", (128, 512)) for i in range(3)]
        xt = [sb(f"xt{i}", (128, 1024)) for i in range(2)]
        rr = [sb(f"rr{i}", (128, 1024)) for i in range(2)]
        st6 = sb("st6", (128, 2, 6)); mv = sb("mv", (128, 4))
        pb = [ps(f"pb{i}", (128, 512)) for i in range(6)]
        pm = ps("pm", (128, 1024))
        nj = 0; ntl = 0
        for (t0, n) in GROUPS:
            if last and t0 < NCTX:
                continue
            v = 1 if t0 < NCTX else 0
            for i, nm in enumerate(("YHG", "YCONV", "YATT")):
                b.ld(Y[i][:, :, 0:n], dr[nm][:, t0:t0 + n].rearrange("(k p) t -> p k t", p=128))
            b.ld(MGs[:, :, 0:n], dr["MG"][:, t0:t0 + n].rearrange("(c p) t -> p c t", p=128))
            for c in range(8):
                pbs = pb[3 * (nj % 2):3 * (nj % 2) + 3]; nj += 1
                for i in range(3):
                    for k in range(8):
                        b.mm(pbs[i][:, 0:n], W[i][:, k, c * 128:(c + 1) * 128], Y[i][:, k, 0:n], start=(k == 0), stop=(k == 7))
                for i in range(3):
                    b.tt("dve", tm[i][:, 0:n], pbs[i][:, 0:n], MGs[:, i * 8 + c, 0:n], ALU.mult)
                b.tt("pool", tm[0][:, 0:n], tm[0][:, 0:n], tm[1][:, 0:n], ALU.add)
                b.tt("pool", K(mrg[:, c, 0:n], f"mrg{c}"), tm[0][:, 0:n], tm[2][:, 0:n], ALU.add)
            for tt_ in range(n // 128):
                par = ntl % 2; ntl += 1
                r0 = t0 + tt_ * 128
                x_ = xt[par]; r_ = rr[par]
                b.ld(x_[:], src[r0:r0 + 128, :])
                for nn in range(2):
                    for k in range(8):
                        b.mm(K(pm[:, nn * 512:(nn + 1) * 512], f"pm{nn}"), K(mrg[:, k, tt_ * 128:(tt_ + 1) * 128], f"mrg{k}"),
                             W[3][:, k, nn * 512:(nn + 1) * 512], start=(k == 0), stop=(k == 7))
                for nn in range(2):
                    b.tt("dve", r_[:, nn * 512:(nn + 1) * 512], K(pm[:, nn * 512:(nn + 1) * 512], f"pm{nn}"),
                         MODBC[:, v, 0, nn * 512:(nn + 1) * 512], ALU.mult)
                b.stt(r_[:], x_[:], ALPHA, r_[:], ALU.mult, ALU.add)
                ln_tile(b, r_, r_, lng, lnb, st6, mv, epsc)
                b.ld(dr["X1"][r0:r0 + 128, :], r_[:], q="pool")
        p.barrier()


def phase_tab(st):
    b = st["b"]; nc, p, dr = b.nc, b.p, b.dr
    with ExitStack() as es:
        sb = lambda n, s, dt=F32: st["sb"](n + "_tab", s, dt, es)
        tin = [sb(f"tin{i}", (128, 4, 1024)) for i in range(3)]
        tout = [sb(f"tout{i}", (128, 4, 1024), BF16) for i in range(3)]
        it = 0
        for tbl, col in (("peer_u", 0), ("peer_v", 1024)):
            srcT = dr[tbl].rearrange("l e d -> (l e) d")
            for blk in range(2 * 16384 // 512):
                r0 = blk * 512
                ti_, to_ = tin[it % 3], tout[it % 3]
                b.ld(ti_[:], srcT[r0:r0 + 512, :].rearrange("(p r) d -> p r d", r=4))
                b.cp("dve" if it % 2 == 0 else "act", to_[:], ti_[:])
                b.ld(dr["UV16"][r0:r0 + 512, col:col + 1024].rearrange("(p r) d -> p r d", r=4), to_[:], q="pool")
                it += 1
        p.barrier()


NGB = 12
GEL = 4


def phase_i(st, l, last, src, dst):
    b = st["b"]; nc, p, dr = b.nc, b.p, b.dr
    ident, MODBC, epsc = st["ident"], st["MODBC"], st["epsc"]
    with ExitStack() as es:
        sb = lambda n, s, dt=F32: st["sb"](n + f"_i{l}", s, dt, es)
        ps = lambda n, s, dt=F32: st["ps"](n + f"_i{l}", s, dt, es)
        wqs = [sb(f"wqs{i}", (128, 8, 512)) for i in range(2)]
        kraw = sb("kraw", (128, 2, 128)); kT = sb("kT", (128, 2, 128))
        b.ld(kraw[:, 0, :], dr["peer_k1"][l]); b.ld(kraw[:, 1, :], dr["peer_k2"][l])
        pT = [ps(f"pT{i}", (128, 512)) for i in range(2)]
        for i in range(2):
            b.tr(pT[0][:, i * 128:(i + 1) * 128], kraw[:, i, :], ident[:])
        b.cp("dve", kT[:], pT[0][:, 0:256].rearrange("p (a c) -> p a c", c=128))
        lng = sb("lng", (128, 1024)); lnb = sb("lnb", (128, 1024)); io = sb("io", (128, 16))
        b.ld(lng[:], dr["ln2_g"][l].partition_broadcast(128), q="pool")
        b.ld(lnb[:], dr["ln2_b"][l].partition_broadcast(128), q="pool")
        b.ld(io[:], dr["iota256"][:, 0:16])
        X1 = [sb(f"x1{i}", (128, 1024)) for i in range(2)]
        HH = [sb(f"hh{i}", (128, 1024)) for i in range(2)]
        HH16 = [sb(f"hh16{i}", (128, 1024), BF16) for i in range(2)]
        EIDs = [sb(f"EID{i}", (128, 128), I32) for i in range(2)]
        GTS = [sb(f"GTs{i}", (128, 8, 16)) for i in range(2)]
        hT = sb("hT", (128, 8, 128))
        qT = [sb(f"qT{i}", (128, 4, 128)) for i in range(2)]
        SC = sb("SC", (128, 16, 128)); SC2 = sb("SC2", (128, 16, 128))
        oh = sb("oh", (128, 8, 16, 16))
        V1 = sb("V1", (128, 16, 16)); I1u = sb("I1u", (128, 16, 16), U32); I1f = sb("I1f", (128, 16, 16))
        SCO = sb("SCO", (128, 8, 16)); CIu = sb("CIu", (128, 8, 16), U32)
        hiU = sb("hiU", (128, 8, 16), U32); loU = sb("loU", (128, 8, 16), U32)
        hiF = sb("hiF", (128, 8, 16)); loF = sb("loF", (128, 8, 16))
        s1 = sb("s1", (128, 8, 16)); s2 = sb("s2", (128, 8, 16))
        SEL = sb("SEL", (128, 128))
        Z = sb("Z", (128, 8)); ACTV = sb("ACTV", (128, 128)); t1 = sb("t1", (128, 128))
        Wt = sb("Wt", (128, 128))
        gb = [sb(f"gb{i}", (128, 2048), BF16) for i in range(NGB)]
        junk = sb("junk", (128, 1024), BF16); acc = sb("acc", (128, 1024))
        st6 = sb("st6", (128, 2, 6)); mv = sb("mv", (128, 4))
        pq = [ps(f"pq{i}", (128, 512)) for i in range(2)]
        psS = [ps(f"psS{i}", (128, 512)) for i in range(2)]
        pv = ps("pv", (128, 1024))
        identr = sb("identr", (128, 128), F32R)
        b.cp("dve", identr[:], ident[:])
        dgs = [sb(f"dgs{i}", (128, 128), BF16) for i in range(4)]
        identb = st["identb"]

        def route(ti, par):
            v = 1 if ti < 2 else 0
            r0 = ti * 128
            x1, hh, EID, GT_ = X1[par], HH[par], EIDs[par], GTS[par]
            b.ld(x1[:], dr["X1"][r0:r0 + 128, :]); yield
            b.tt("dve", hh[:], x1[:], MODBC[:, v, 3, :], ALU.mult); yield
            b.tt("pool", hh[:], hh[:], MODBC[:, v, 2, :], ALU.add); yield
            b.cp("pool", HH16[par][:], hh[:]); yield
            for hf in range(2):
                for j in range(4):
                    k = hf * 4 + j
                    b.tr(pT[hf][:, j * 128:(j + 1) * 128], hh[:, k * 128:(k + 1) * 128], ident[:])
                b.cp("act", hT[:, hf * 4:(hf + 1) * 4, :], pT[hf][:].rearrange("p (a c) -> p a c", c=128)); yield
            for g4 in range(4):
                pq_ = pq[g4 % 2]; qt = qT[g4 % 2]; pss = psS[g4 % 2]
                wq_ = wqs[g4 % 2]
                b.ld(wq_[:], dr["peer_wq"][l, :, g4 * 512:(g4 + 1) * 512].rearrange("(k p) c -> p k c", p=128)); yield
                for j in range(4):
                    cc = g4 * 4 + j
                    for k in range(8):
                        b.mm(pq_[:, j * 128:(j + 1) * 128], wq_[:, k, j * 128:(j + 1) * 128], hT[:, k, :],
                             start=(k == 0), stop=(k == 7))
                b.cp("act", qt[:], pq_[:].rearrange("p (a c) -> p a c", c=128)); yield
                for j in range(4):
                    cc = g4 * 4 + j
                    b.mm(pss[:, j * 128:(j + 1) * 128], qt[:, j, :], kT[:, cc % 2, :])
                b.cp("act", SC[:, g4 * 4:(g4 + 1) * 4, :], pss[:].rearrange("p (a c) -> p a c", c=128)); yield
            kV = lambda cc: f"V1_{cc}"
            for cc in range(16):
                p.op("dve", lambda: nc.vector.max(V1[:, cc, 0:8], SC[:, cc, :]), [SC[:]], [kV(cc)]); yield
            for cc in range(16):
                p.op("dve", lambda: nc.vector.max_index(I1u[:, cc, 0:8], V1[:, cc, 0:8], SC[:, cc, :]), [SC[:], kV(cc)], [f"I1_{cc}"]); yield
            for cc in range(16):
                p.op("dve", lambda: nc.vector.match_replace(SC2[:, cc, :], V1[:, cc, 0:8], SC[:, cc, :], -1e30), [SC[:], kV(cc)], [f"SC2_{cc}"]); yield
            for cc in range(16):
                p.op("dve", lambda: nc.vector.max(V1[:, cc, 8:16], SC2[:, cc, :]), [f"SC2_{cc}"], [f"V1b_{cc}"]); yield
            for cc in range(16):
                p.op("dve", lambda: nc.vector.max_index(I1u[:, cc, 8:16], V1[:, cc, 8:16], SC2[:, cc, :]), [f"SC2_{cc}", f"V1b_{cc}"], [I1u[:], V1[:], SC2[:]]); yield
            b.cp("dve", I1f[:], I1u[:]); yield
            cnd = SC[:].rearrange("p a c -> p (a c)").rearrange("p (h i j) -> p h i j", h=8, i=16)
            for h in range(8):
                b.tt("dve", cnd[:, h], V1[:, 2 * h, :, None].to_broadcast([128, 16, 16]),
                     V1[:, 2 * h + 1, None, :].to_broadcast([128, 16, 16]), ALU.add); yield
            cflat = SC[:].rearrange("p a c -> p (a c)").rearrange("p (h c) -> p h c", h=8)
            C2 = SC2[:].rearrange("p a c -> p (a c)").rearrange("p (h c) -> p h c", h=8)
            for h in range(8):
                p.op("dve", lambda: nc.vector.max(SCO[:, h, 0:8], cflat[:, h, :]), [SC[:]], [f"SCO_{h}"]); yield
            for h in range(8):
                p.op("dve", lambda: nc.vector.max_index(CIu[:, h, 0:8], SCO[:, h, 0:8], cflat[:, h, :]), [SC[:], f"SCO_{h}"], [f"CI_{h}"]); yield
            for h in range(8):
                p.op("dve", lambda: nc.vector.match_replace(C2[:, h, :], SCO[:, h, 0:8], cflat[:, h, :], -1e30), [SC[:], f"SCO_{h}"], [f"C2_{h}"]); yield
            for h in range(8):
                p.op("dve", lambda: nc.vector.max(SCO[:, h, 8:16], C2[:, h, :]), [f"C2_{h}"], [f"SCOb_{h}"]); yield
            for h in range(8):
                p.op("dve", lambda: nc.vector.max_index(CIu[:, h, 8:16], SCO[:, h, 8:16], C2[:, h, :]), [f"C2_{h}", f"SCOb_{h}"], [CIu[:], SCO[:], SC2[:]]); yield
            p.op("dve", lambda: nc.vector.tensor_single_scalar(hiU[:], CIu[:], 4, ALU.logical_shift_right), [CIu[:]], [hiU[:]]); yield
            p.op("dve", lambda: nc.vector.tensor_single_scalar(loU[:], CIu[:], 15, ALU.bitwise_and), [CIu[:]], [loU[:]]); yield
            b.cp("dve", hiF[:], hiU[:]); yield
            b.cp("dve", loF[:], loU[:]); yield
            I1v = I1f[:].rearrange("p (h a) i -> p h a i", a=2)
            iob = io[:, None, None, :].to_broadcast([128, 8, 16, 16])
            for (xf, half, so) in ((hiF, 0, s1), (loF, 1, s2)):
                b.tt("dve", oh[:], iob, xf[:, :, :, None].to_broadcast([128, 8, 16, 16]), ALU.is_equal); yield
                b.tt("dve", oh[:], oh[:], I1v[:, :, half, None, :].to_broadcast([128, 8, 16, 16]), ALU.mult); yield
                p.op("dve", lambda: nc.vector.tensor_reduce(so[:], oh[:], AX.X, ALU.add), [oh[:]], [so[:]]); yield
            b.stt(SEL[:], s1[:].rearrange("p a b -> p (a b)"), 128.0, s2[:].rearrange("p a b -> p (a b)"), ALU.mult, ALU.add); yield
            if l > 0:
                b.ts("dve", SEL[:], SEL[:], float(l * 16384), None, ALU.add); yield
            b.cp("dve", EID[:], SEL[:]); yield
            b.tt("dve", GT_[:], SCO[:], SCO[:, :, 0:1].to_broadcast([128, 8, 16]), ALU.subtract); yield
            b.act(GT_[:], GT_[:], AF.Exp); yield
            p.op("dve", lambda: nc.vector.tensor_reduce(Z[:], GT_[:], AX.X, ALU.add), [GT_[:]], [Z[:]]); yield
            b.recip(Z[:], Z[:]); yield
            b.tt("dve", GT_[:], GT_[:], Z[:, :, None].to_broadcast([128, 8, 16]), ALU.mult); yield

        ngc = [0]

        def experts(ti, par, nxt):
            v = 1 if ti < 2 else 0
            r0 = ti * 128
            x1, hh, EID, GT_ = X1[par], HH[par], EIDs[par], GTS[par]
            step = (lambda: next(nxt, None)) if nxt is not None else (lambda: None)
            hh16 = HH16[par]
            gtf = GT_[:].rearrange("p h k -> p (h k)")
            for s0 in range(0, 128, GEL):
                slots = []
                for s in range(s0, s0 + GEL):
                    g_ = gb[ngc[0] % NGB]; ngc[0] += 1
                    slots.append(g_)
                    p.gather(g_[:], dr["UV16"], EID[:, s:s + 1])
                    b.stt(junk[:], g_[:, 0:1024], 1.0, hh16[:], ALU.mult, ALU.mult, accum_out=ACTV[:, s:s + 1])
                    step()
                cs = slice(s0, s0 + GEL)
                b.act(t1[:, cs], ACTV[:, cs], AF.Gelu_apprx_tanh)
                b.tt("dve", Wt[:, cs], t1[:, cs], gtf[:, cs], ALU.mult)
                for s, g_ in zip(range(s0, s0 + GEL), slots):
                    dg_ = dgs[s % 4]
                    b.act(dg_[:], identb[:], AF.Copy, scale=Wt[:, s:s + 1])
                    for nn in range(2):
                        b.mm(K(pv[:, nn * 512:(nn + 1) * 512], f"pv{nn}"), dg_[:], g_[:, 1024 + nn * 512:1024 + (nn + 1) * 512],
                             start=(s == 0), stop=(s == 127))
            for nn in range(2):
                b.tt("dve", acc[:, nn * 512:(nn + 1) * 512], K(pv[:, nn * 512:(nn + 1) * 512], f"pv{nn}"),
                     MODBC[:, v, 1, nn * 512:(nn + 1) * 512], ALU.mult)
            b.stt(acc[:], x1[:], ALPHA, acc[:], ALU.mult, ALU.add)
            ln_tile(b, acc, acc, lng, lnb, st6, mv, epsc)
            if last:
                b.ld(dst[r0 - NCTX:r0 - NCTX + 128, :], acc[:], q="sp")
            else:
                b.ld(dst[r0:r0 + 128, :], acc[:], q="sp")

        tiles = [ti for ti in range(NTILE) if not (last and ti < 2)]
        g0 = route(tiles[0], 0)
        for _ in g0:
            pass
        for idx, ti in enumerate(tiles):
            nxt = route(tiles[idx + 1], (idx + 1) % 2) if idx + 1 < len(tiles) else None
            experts(ti, idx % 2, nxt)
            if nxt is not None:
                for _ in nxt:
                    pass
        p.barrier()


_CACHE = {}


def kernel(**inputs):
    if "bld" not in _CACHE:
        _CACHE["bld"] = build()
    bld = _CACHE["bld"]
    consts = host_consts()
    rope_full = consts.pop("ropecs_full")
    f32 = lambda a: np.ascontiguousarray(np.asarray(a, dtype=np.float32))
    shared = {n: f32(inputs[n]) for n, _ in WNAMES}
    shared.update(consts)
    x, c, ctx, c_ctx = (f32(inputs[k]) for k in ("x", "c", "ctx", "c_ctx"))
    nb = x.shape[0]
    in_maps = []
    for core in range(8):
        bi, role = (core // 2) % nb, core % 2
        m = dict(shared)
        m["xin"] = np.ascontiguousarray(np.concatenate([ctx[bi], x[bi, role * NX:(role + 1) * NX]], axis=0))
        m["cvec"] = np.ascontiguousarray(np.concatenate([c[bi].reshape(8, 128), c_ctx.reshape(8, 128)], axis=0))
        m["ropecs"] = np.ascontiguousarray(rope_full[role * NX:(role + 1) * NX])
        rm = np.zeros((128, 2), np.float32)
        rm[:, 0] = float(role == 1)
        rm[:, 1] = float(role == 0)
        m["rolem"] = rm
        in_maps.append(m)
    res = run_bass_kernel_spmd(bld.nc, in_maps, core_ids=list(range(8)))
    out = np.empty((nb, NXF, D), np.float32)
    for core in range(2 * nb):
        bi, role = core // 2, core % 2
        out[bi, role * NX:(role + 1) * NX] = np.asarray(res.results[core]["y"], dtype=np.float32)
    return out
```
